# Optimizing a Trainium2 kernel written in Bass

```python
import math
import jax, jax.numpy as jnp
from jax import lax
import numpy as np

D_MODEL = 1024
BATCH = 16
SEQ = 2048
DEPTH = 4

N_MIXERS = 2
N_HGRN_LAYERS = (DEPTH + 1) // 2
N_DSA_LAYERS = DEPTH // 2

HGRN_EXPAND = 128
HGRN_HEADS = D_MODEL // HGRN_EXPAND
HGRN_DK = HGRN_EXPAND
HGRN_DV = D_MODEL // HGRN_HEADS
HGRN_CHUNK = 32

ATT_HEADS = 16
ATT_HEAD_DIM = D_MODEL // ATT_HEADS
ATT_KV_HEADS = 2
ATT_GROUP = ATT_HEADS // ATT_KV_HEADS
IDX_HEADS = 8
IDX_HEAD_DIM = 64
TOPK_MAX = 256
Q_BLOCK = 128
DSA_SPLITS = (ATT_HEADS * ATT_HEAD_DIM, ATT_KV_HEADS * ATT_HEAD_DIM, ATT_KV_HEADS * ATT_HEAD_DIM,
              IDX_HEADS * IDX_HEAD_DIM, IDX_HEAD_DIM, IDX_HEADS)
DSA_IN_WIDTH = sum(DSA_SPLITS)

REL_BUCKETS = 32
REL_MAX_DIST = 128

D_FF = 2816
CONV_WIDTH = 3

EPS = 1e-6
NEG_BIG = -1e30
TINY = 1e-30

kernel_name = "hybrid_hgrn2_dsa_convffn_trunk"


def rms_norm(x, gain):
    xf = x.astype(jnp.float32)
    y = xf * lax.rsqrt(jnp.mean(xf * xf, axis=-1, keepdims=True) + EPS)
    return (y * gain.astype(jnp.float32)).astype(x.dtype)


def hgrn2_mixer(x, w_in, w_out, gate_norm, lower_bound):
    B, L, _ = x.shape
    n_chunks = L // HGRN_CHUNK
    proj = x @ w_in
    q, f, i, g = jnp.split(proj, 4, axis=-1)
    f32 = f.astype(jnp.float32)
    lb = lower_bound.astype(jnp.float32)
    forget = lb + (1.0 - lb) * jax.nn.sigmoid(f32)
    log_f = jnp.log(jnp.maximum(forget, TINY))
    k = (1.0 - lb) * jax.nn.sigmoid(-f32)

    def to_chunks(t, d):
        t = t.astype(jnp.float32).reshape(B, n_chunks, HGRN_CHUNK, HGRN_HEADS, d)
        return t.transpose(1, 0, 3, 2, 4)

    qc, kc, ic, gc = to_chunks(q, HGRN_DK), to_chunks(k, HGRN_DK), to_chunks(i, HGRN_DV), to_chunks(log_f, HGRN_DK)
    causal = jnp.tril(jnp.ones((HGRN_CHUNK, HGRN_CHUNK), dtype=bool))[None, None, :, :, None]

    def step(S, inp):
        qb, kb, ib, gb = inp
        b = jnp.cumsum(gb, axis=2)
        diff = b[:, :, :, None, :] - b[:, :, None, :, :]
        decay = jnp.where(causal, jnp.exp(jnp.where(causal, diff, 0.0)), 0.0)
        scores = jnp.einsum('bhtd,bhsd,bhtsd->bhts', qb, kb, decay)
        o = jnp.einsum('bhts,bhsv->bhtv', scores, ib) + jnp.einsum('bhtd,bhdv->bhtv', qb * jnp.exp(b), S)
        b_last = b[:, :, -1:, :]
        S_new = jnp.exp(b_last[:, :, 0, :])[..., None] * S + jnp.einsum('bhsd,bhsv->bhdv', kb * jnp.exp(b_last - b), ib)
        return S_new, o

    S0 = jnp.zeros((B, HGRN_HEADS, HGRN_DK, HGRN_DV), jnp.float32)
    _, o = lax.scan(step, S0, (qc, kc, ic, gc))
    o = o.transpose(1, 0, 3, 2, 4).reshape(B, L, HGRN_HEADS, HGRN_DV)
    o = o * lax.rsqrt(jnp.mean(o * o, axis=-1, keepdims=True) + EPS) * gate_norm.astype(jnp.float32)
    o = o * jax.nn.silu(g.astype(jnp.float32)).reshape(B, L, HGRN_HEADS, HGRN_DV)
    return o.reshape(B, L, D_MODEL).astype(x.dtype) @ w_out


def t5_causal_bucket(rel):
    n = jnp.maximum(rel, 0)
    max_exact = REL_BUCKETS // 2
    nf = jnp.maximum(n, max_exact).astype(jnp.float32)
    large = max_exact + (jnp.log(nf / max_exact) / math.log(REL_MAX_DIST / max_exact)
                         * (REL_BUCKETS - max_exact)).astype(jnp.int32)
    large = jnp.minimum(large, REL_BUCKETS - 1)
    return jnp.where(n < max_exact, n, large)


def dsa_mixer(x, w_in, w_out, rel_bias):
    B, L, _ = x.shape
    top_k = min(TOPK_MAX, L // 4)
    n_blocks = L // Q_BLOCK
    proj = x @ w_in
    cuts = np.cumsum(DSA_SPLITS)[:-1].tolist()
    q, k, v, q_idx, k_idx, w_idx = jnp.split(proj, cuts, axis=-1)
    q = q.reshape(B, L, ATT_KV_HEADS, ATT_GROUP, ATT_HEAD_DIM)
    q_idx = q_idx.reshape(B, L, IDX_HEADS, IDX_HEAD_DIM)
    w_idx = w_idx * (IDX_HEADS ** -0.5 * IDX_HEAD_DIM ** -0.5)

    def blockify(t):
        return jnp.moveaxis(t.reshape(B, n_blocks, Q_BLOCK, *t.shape[2:]), 1, 0)

    key_pos = jnp.arange(L, dtype=jnp.int32)
    starts = jnp.arange(n_blocks, dtype=jnp.int32) * Q_BLOCK

    def one_block(args):
        qb, qib, wb, start = args
        q_pos = start + jnp.arange(Q_BLOCK, dtype=jnp.int32)
        dots = jnp.einsum('bthd,bsd->bths', qib, k_idx).astype(jnp.float32)
        scores = jnp.einsum('bths,bth->bts', jax.nn.relu(dots), wb.astype(jnp.float32))
        admissible = key_pos[None, :] <= q_pos[:, None]
        scores = jnp.where(admissible[None], scores, NEG_BIG)
        _, sel = lax.top_k(scores, top_k)
        k_sel = jax.vmap(lambda kk, ii: kk[ii])(k, sel).reshape(B, Q_BLOCK, top_k, ATT_KV_HEADS, ATT_HEAD_DIM)
        v_sel = jax.vmap(lambda vv, ii: vv[ii])(v, sel).reshape(B, Q_BLOCK, top_k, ATT_KV_HEADS, ATT_HEAD_DIM)
        logits = jnp.einsum('btkgd,btskd->btkgs', qb, k_sel).astype(jnp.float32) * (ATT_HEAD_DIM ** -0.5)
        rel = q_pos[None, :, None] - sel
        bias = rel_bias.astype(jnp.float32)[t5_causal_bucket(rel)]
        bias = bias.reshape(B, Q_BLOCK, top_k, ATT_KV_HEADS, ATT_GROUP).transpose(0, 1, 3, 4, 2)
        valid = (rel >= 0)[:, :, None, None, :]
        logits = jnp.where(valid, logits + bias, NEG_BIG)
        probs = jax.nn.softmax(logits, axis=-1)
        out = jnp.einsum('btkgs,btskd->btkgd', probs, v_sel.astype(jnp.float32))
        return out.reshape(B, Q_BLOCK, D_MODEL).astype(x.dtype)

    out = lax.map(one_block, (blockify(q), blockify(q_idx), blockify(w_idx), starts))
    out = jnp.moveaxis(out, 0, 1).reshape(B, L, D_MODEL)
    return out @ w_out


def conv_ffn(x, w_up, conv_w, conv_b, w_down):
    h = x @ w_up
    C = h.shape[-1]
    h = lax.conv_general_dilated(h, conv_w[:, None, :].astype(h.dtype), window_strides=(1,),
                                 padding=[(CONV_WIDTH - 1, 0)],
                                 dimension_numbers=('NWC', 'WIO', 'NWC'),
                                 feature_group_count=C) + conv_b
    gate, up = jnp.split(h, 2, axis=-1)
    return (jax.nn.silu(gate) * up) @ w_down


def setup_inputs(seed: int = 0) -> dict:
    key = jax.random.key(seed)
    ks = jax.random.split(key, 16)
    D = D_MODEL

    def dense(k, shape, fan_in):
        return jax.random.normal(k, shape, jnp.float32) * fan_in ** -0.5

    return {
        "x": jax.random.normal(ks[0], (BATCH, SEQ, D), jnp.float32),
        "attn_norm": 1.0 + 0.05 * jax.random.normal(ks[1], (DEPTH, D), jnp.float32),
        "ffn_norm": 1.0 + 0.05 * jax.random.normal(ks[2], (DEPTH, D), jnp.float32),
        "hgrn_w_in": dense(ks[3], (N_HGRN_LAYERS, D, 4 * D), D),
        "hgrn_w_out": dense(ks[4], (N_HGRN_LAYERS, D, D), D),
        "hgrn_gate_norm": 1.0 + 0.05 * jax.random.normal(ks[5], (N_HGRN_LAYERS, HGRN_DV), jnp.float32),
        "hgrn_lower_bounds": 0.1 * jax.random.normal(ks[6], (N_HGRN_LAYERS, D), jnp.float32),
        "dsa_w_in": dense(ks[7], (N_DSA_LAYERS, D, DSA_IN_WIDTH), D),
        "dsa_w_out": dense(ks[8], (N_DSA_LAYERS, D, D), D),
        "rel_bias": 0.1 * jax.random.normal(ks[9], (REL_BUCKETS, ATT_HEADS), jnp.float32),
        "ffn_w_up": dense(ks[10], (DEPTH, D, 2 * D_FF), D),
        "ffn_conv_w": dense(ks[11], (DEPTH, CONV_WIDTH, 2 * D_FF), CONV_WIDTH),
        "ffn_conv_b": 0.01 * jax.random.normal(ks[12], (DEPTH, 2 * D_FF), jnp.float32),
        "ffn_w_down": dense(ks[13], (DEPTH, D_FF, D), D_FF),
        "final_norm": 1.0 + 0.05 * jax.random.normal(ks[14], (D,), jnp.float32),
    }


def reference(x, attn_norm, ffn_norm, hgrn_w_in, hgrn_w_out, hgrn_gate_norm, hgrn_lower_bounds,
              dsa_w_in, dsa_w_out, rel_bias, ffn_w_up, ffn_conv_w, ffn_conv_b, ffn_w_down, final_norm):
    lb_soft = jax.nn.softmax(hgrn_lower_bounds.astype(jnp.float32), axis=0)
    lower_bounds = jnp.cumsum(lb_soft, axis=0) - lb_soft[0]
    h = x
    for layer in range(DEPTH):
        j = layer // N_MIXERS
        hn = rms_norm(h, attn_norm[layer])
        if layer % N_MIXERS == 0:
            mix = hgrn2_mixer(hn, hgrn_w_in[j], hgrn_w_out[j], hgrn_gate_norm[j], lower_bounds[j])
        else:
            mix = dsa_mixer(hn, dsa_w_in[j], dsa_w_out[j], rel_bias)
        h = h + mix
        h = h + conv_ffn(rms_norm(h, ffn_norm[layer]), ffn_w_up[layer], ffn_conv_w[layer],
                         ffn_conv_b[layer], ffn_w_down[layer])
    return rms_norm(h, final_norm)
```

```python
import math
from contextlib import ExitStack
import numpy as np
import concourse.bass as bass
import concourse.mybir as mybir
from concourse.bass_utils import run_bass_kernel_spmd

F32 = mybir.dt.float32
BF16 = mybir.dt.bfloat16
AF = mybir.ActivationFunctionType
ALU = mybir.AluOpType
AX = mybir.AxisListType

D = 1024
L = 2048
ST = 512
NST = L // ST
DFF = 2816
NSEQ = 2
DEPTH = 4
EPS = 1e-6
NEG = -1.0e30
BIG = 30000.0
EPOCH = 24000


class Buf:
    __slots__ = ("name", "w", "rs")

    def __init__(self, name=""):
        self.name = name
        self.w = None
        self.rs = {}


class Ctx:
    ENG = ("pe", "act", "dve", "pool", "sp")

    def __init__(self, nc, es, n_dma_sems=32, n_epochs=6):
        self.nc = nc
        self.e = {"pe": nc.tensor, "act": nc.scalar, "dve": nc.vector, "pool": nc.gpsimd, "sp": nc.sync}
        self.sem = {n: [es.enter_context(nc.semaphore(f"s_{n}_{k}")) for k in range(n_epochs)] for n in self.ENG}
        self.cnt = {n: 0 for n in self.ENG}
        self.seen = {n: {} for n in self.ENG}
        self.dsem = [es.enter_context(nc.semaphore(f"s_dma_{k}")) for k in range(n_dma_sems)]
        self.dval = [0] * n_dma_sems
        self.dnext = 0
        self.nwaits = 0
        self.wstat = {}

    def _wait(self, eng, tok):
        if tok is None:
            return
        if tok[0] == "e":
            _, src, idx = tok
            ep, v = divmod(idx - 1, EPOCH)
            v += 1
            seen = self.seen[eng]
            for kk in seen:
                if len(kk) == 3 and kk[1] == src and kk[2] > ep:
                    return
            key = ("e", src, ep)
            if seen.get(key, 0) >= v:
                return
            self.e[eng].wait_ge(self.sem[src][ep], v)
            seen[key] = v
            self.nwaits += 1
            self.wstat[(src, eng)] = self.wstat.get((src, eng), 0) + 1
        else:
            _, k, val = tok
            key = ("d", k)
            if self.seen[eng].get(key, 0) >= val:
                return
            self.e[eng].wait_ge(self.dsem[k], val)
            self.seen[eng][key] = val
            self.nwaits += 1

    def _deps(self, eng, reads, writes, pe_accum=False):
        for b in reads:
            self._wait(eng, b.w)
        for b in writes:
            if not (pe_accum and b.w is not None and b.w[0] == "e" and b.w[1] == "pe"):
                self._wait(eng, b.w)
            for t in b.rs.values():
                self._wait(eng, t)

    def op(self, eng, fn, reads=(), writes=(), pe_accum=False):
        self._deps(eng, reads, writes, pe_accum)
        ins = fn(self.e[eng])
        self.cnt[eng] += 1
        idx = self.cnt[eng]
        ins.then_inc(self.sem[eng][(idx - 1) // EPOCH], 1)
        tok = ("e", eng, idx)
        for b in writes:
            b.w = tok
            b.rs = {}
        for b in reads:
            b.rs[eng] = tok
        return ins

    def dma(self, eng, out, in_, reads=(), writes=(), **kw):
        k = self.dnext
        self.dnext = (self.dnext + 1) % len(self.dsem)
        if self.dval[k] > 0:
            self._wait(eng, ("d", k, self.dval[k]))
        self._deps(eng, reads, writes)
        self.dval[k] += 16
        ins = self.e[eng].dma_start(out=out, in_=in_, **kw)
        ins.then_inc(self.dsem[k], 16)
        tok = ("d", k, self.dval[k])
        for b in writes:
            b.w = tok
            b.rs = {}
        for b in reads:
            b.rs[("d", k)] = tok
        return tok

    def release(self, bufs):
        for eng in self.ENG:
            for b in bufs:
                self._wait(eng, b.w)
                for t in b.rs.values():
                    self._wait(eng, t)


class Tl:
    def __init__(self, t, name=""):
        self.t = t
        self.b = Buf(name)


def _t5_bucket(n):
    n = max(int(n), 0)
    if n < 16:
        return n
    nf = np.float32(max(n, 16))
    v = np.float32(np.log(nf / np.float32(16))) / np.float32(math.log(128 / 16)) * np.float32(16)
    return min(16 + int(v), 31)


def _host_consts():
    ident = np.eye(128, dtype=np.float32)
    maskbd = np.zeros((128, 128), np.float32)
    for s in range(128):
        for t in range(128):
            if s // 64 == t // 64 and s <= t:
                maskbd[s, t] = 1.0
    scanmask = np.ones((128, 512), np.float32)
    scanmask[:, ::64] = 0.0
    gr = np.zeros((32, 383), np.float32)
    for u in range(383):
        gr[_t5_bucket(255 - u), u] = 1.0
    causal = np.zeros((128, 128), np.float32)
    for t in range(128):
        causal[t, t + 1:] = NEG
    return {"c_ident": ident, "c_maskbd": maskbd, "c_scanmask": scanmask, "c_gr": gr, "c_causal": causal}


def build_nc(depth=DEPTH, nseq=NSEQ, plan=None, tiny=(), dbg_stop=99):
    if plan is None:
        plan = []
        for layer in range(depth):
            plan.append(("hgrn" if layer % 2 == 0 else "dsa", layer))
            plan.append(("ffn", layer))
    nc = bass.Bass("TRN2", target_bir_lowering=False)
    dt = lambda name, shape, kind="ExternalInput": nc.dram_tensor(name, [1, 1, 1] if name in tiny else list(shape), F32, kind=kind).ap()
    x = dt("x", [NSEQ, L, D])
    attn_norm = dt("attn_norm", [4, D]); ffn_norm = dt("ffn_norm", [4, D])
    hgrn_w_in = dt("hgrn_w_in", [2, D, 4 * D]); hgrn_w_out = dt("hgrn_w_out", [2, D, D])
    hgrn_gate_norm = dt("hgrn_gate_norm", [2, 128]); hgrn_lb = dt("hgrn_lower_bounds", [2, D])
    dsa_w_in = dt("dsa_w_in", [2, D, 1864]); dsa_w_out = dt("dsa_w_out", [2, D, D])
    rel_bias = dt("rel_bias", [32, 16])
    ffn_w_up = dt("ffn_w_up", [4, D, 2 * DFF]); ffn_conv_w = dt("ffn_conv_w", [4, 3, 2 * DFF])
    ffn_conv_b = dt("ffn_conv_b", [4, 2 * DFF]); ffn_w_down = dt("ffn_w_down", [4, DFF, D])
    final_norm = dt("final_norm", [D])
    c_ident = dt("c_ident", [128, 128]); c_maskbd = dt("c_maskbd", [128, 128])
    c_scanmask = dt("c_scanmask", [128, 512]); c_gr = dt("c_gr", [32, 383]); c_causal = dt("c_causal", [128, 128])
    out = dt("out", [NSEQ, L, D], kind="ExternalOutput")

    with ExitStack() as es:
        c = Ctx(nc, es)
        op = c.op

        uniq = [0]

        def sb(stack, name, shape, dtype=F32):
            uniq[0] += 1
            name = f"{name}_{uniq[0]}"
            return Tl(stack.enter_context(nc.sbuf_tensor(name, list(shape), dtype)), name)

        P = es.enter_context(nc.psum_tensor("P", [128, 8, 512], F32))
        pb = [Buf(f"pb{i}") for i in range(8)]

        hT = es.enter_context(nc.sbuf_tensor("hT", [128, 8, L], F32))
        bh = [Buf(f"h{st}") for st in range(NST)]
        ident = sb(es, "ident", [128, 128]); onesb = sb(es, "onesb", [128, 128], BF16)
        maskbd = sb(es, "maskbd", [128, 128]); scanmask = sb(es, "scanmask", [128, 512])
        gr = sb(es, "gr", [32, 383]); rb = sb(es, "rb", [32, 16]); cb = sb(es, "cb", [128, 16])
        causal = sb(es, "causal", [128, 128])
        vec = sb(es, "vec", [128, 88])
        convw = sb(es, "convw", [128, 528]); convb = sb(es, "convb", [128, 176])
        gnb = sb(es, "gnb", [128, 2, 128])
        lbt = sb(es, "lbt", [128, 2, 8]); omlt = sb(es, "omlt", [128, 2, 8]); nomlt = sb(es, "nomlt", [128, 2, 8])
        hn = sb(es, "hn", [128, 8, ST], BF16)
        sq = [sb(es, f"sq{i}", [128, ST], BF16) for i in range(2)]
        rstd = sb(es, "rstd", [128, ST])
        wbufs = [sb(es, f"wbuf{i}", [128, 8, 512], BF16) for i in range(3)]
        wsub = [[Buf(f"w{i}_{k}") for k in range(8)] for i in range(3)]
        wstate = {"i": 0}

        def wnext():
            i = wstate["i"]
            wstate["i"] = (i + 1) % 3
            return wbufs[i].t, wsub[i]

        def wload(dst, dram_ap, bufs):
            c.dma("pool", dst, dram_ap, writes=bufs)

        c.dma("sp", ident.t[:], c_ident, writes=[ident.b])
        c.dma("sp", maskbd.t[:], c_maskbd, writes=[maskbd.b])
        c.dma("sp", scanmask.t[:], c_scanmask, writes=[scanmask.b])
        c.dma("sp", gr.t[:], c_gr, writes=[gr.b])
        c.dma("sp", causal.t[:], c_causal, writes=[causal.b])
        c.dma("sp", rb.t[:], rel_bias, writes=[rb.b])
        c.dma("sp", cb.t[:], rel_bias[31:32, :].partition_broadcast(128) if False else rel_bias[31:32, :].to_broadcast([128, 16]), writes=[cb.b])
        c.dma("sp", gnb.t[:], hgrn_gate_norm.unsqueeze(0).to_broadcast([128, 2, 128]), writes=[gnb.b])
        op("dve", lambda e: e.memset(onesb.t[:], 1.0), writes=[onesb.b])

        with ExitStack() as ps_:
            stg = sb(ps_, "stg", [128, 128])

            def load_vecs(rows_ap, nrows, dst_tl, col0):
                r = 0
                while r < nrows:
                    n = min(128, nrows - r)
                    c.dma("sp", stg.t[0:n, :], rows_ap[r:r + n, :], writes=[stg.b])
                    op("pe", lambda e: e.transpose(P[:, 0, 0:n], stg.t[0:n, :], ident.t[0:n, 0:n]),
                       reads=[stg.b, ident.b], writes=[pb[0]])
                    op("dve", lambda e: e.tensor_copy(dst_tl.t[:, col0 + r:col0 + r + n], P[:, 0, 0:n]),
                       reads=[pb[0]], writes=[dst_tl.b])
                    r += n

            load_vecs(attn_norm.rearrange("l (c p) -> (l c) p", p=128), 32, vec, 0)
            load_vecs(ffn_norm.rearrange("l (c p) -> (l c) p", p=128), 32, vec, 32)
            load_vecs(final_norm.rearrange("(c p) -> c p", p=128), 8, vec, 64)
            load_vecs(hgrn_lb.rearrange("l (c p) -> (l c) p", p=128), 16, vec, 72)
            load_vecs(ffn_conv_w.rearrange("l k (c p) -> (l k c) p", p=128), 528, convw, 0)
            load_vecs(ffn_conv_b.rearrange("l (c p) -> (l c) p", p=128), 176, convb, 0)
            op("dve", lambda e: e.memset(lbt.t[:, 0, :], 0.0), writes=[lbt.b])
            op("dve", lambda e: e.tensor_tensor(lbt.t[:, 1, :], vec.t[:, 80:88], vec.t[:, 72:80], ALU.subtract),
               reads=[vec.b], writes=[lbt.b])
            op("act", lambda e: e.activation(lbt.t[:, 1, :], lbt.t[:, 1, :], AF.Sigmoid), reads=[lbt.b], writes=[lbt.b])
            op("dve", lambda e: e.tensor_scalar(omlt.t[:], lbt.t[:], -1.0, 1.0, ALU.mult, ALU.add), reads=[lbt.b], writes=[omlt.b])
            op("dve", lambda e: e.tensor_scalar(nomlt.t[:], lbt.t[:], 1.0, -1.0, ALU.mult, ALU.add), reads=[lbt.b], writes=[nomlt.b])
            c.release([stg.b])

        def norm(st, gcol0):
            ts = slice(st * ST, (st + 1) * ST)
            for dc in range(8):
                s_ = sq[dc % 2]
                op("act", lambda e: e.activation(s_.t[:], hT[:, dc, ts], AF.Square), reads=[bh[st]], writes=[s_.b])
                op("pe", lambda e: e.matmul(P[:, 3, :], onesb.t[:], s_.t[:], start=(dc == 0), stop=(dc == 7)),
                   reads=[onesb.b, s_.b], writes=[pb[3]], pe_accum=True)
            op("act", lambda e: e.activation(rstd.t[:], P[:, 3, :], AF.Sqrt, bias=EPS, scale=1.0 / D), reads=[pb[3]], writes=[rstd.b])
            op("dve", lambda e: e.reciprocal(rstd.t[:], rstd.t[:]), reads=[rstd.b], writes=[rstd.b])
            for dc in range(8):
                op("dve", lambda e: e.scalar_tensor_tensor(hn.t[:, dc, :], hT[:, dc, ts], vec.t[:, gcol0 + dc:gcol0 + dc + 1],
                                                           rstd.t[:], ALU.mult, ALU.mult),
                   reads=[bh[st], vec.b, rstd.b], writes=[hn.b])

        def mm_fm(bank, wt, wb_, col0, rhs_tl, rhs_ap_fn, nk=8):
            for k in range(nk):
                op("pe", lambda e: e.matmul(P[:, bank, :], wt[:, k, col0:col0 + 128], rhs_ap_fn(k), start=(k == 0), stop=(k == nk - 1)),
                   reads=list(wb_) + [rhs_tl.b], writes=[pb[bank]], pe_accum=True)

        def out_proj(w_dram, src_tl, st, banks=(0, 1)):
            ts = slice(st * ST, (st + 1) * ST)
            it = 0
            tiles = []
            for half in range(2):
                wt, wsb = wnext()
                wload(wt[:], w_dram[:, half * 512:(half + 1) * 512].rearrange("(k p) n -> p k n", p=128), wsb)
                tiles.append((wt, wsb))
            for half in range(2):
                wt, wsb = tiles[half]
                for dq in range(4):
                    dc = 4 * half + dq
                    bank = banks[it % 2]; it += 1
                    mm_fm(bank, wt, wsb, dq * 128, src_tl, lambda k: src_tl.t[:, k, :])
                    op("dve", lambda e: e.tensor_tensor(hT[:, dc, ts], P[:, bank, :], hT[:, dc, ts], ALU.add),
                       reads=[pb[bank], bh[st]], writes=[bh[st]])

        def ffn_phase(layer):
            with ExitStack() as ps_:
                g = sb(ps_, "g", [128, 22, ST], BF16)
                ag = [sb(ps_, f"ag{i}", [128, ST]) for i in range(2)]
                au = [sb(ps_, f"au{i}", [128, ST]) for i in range(2)]
                carry = [sb(ps_, f"carry{i}", [128, 44, 2]) for i in range(2)]
                bnd = sb(ps_, "bnd", [128, 44, 2]); tmpb = sb(ps_, "tmpb", [128, 44])
                wup = ffn_w_up[layer]
                wdn = ffn_w_down[layer]
                cw = lambda k, ci: convw.t[:, (layer * 3 + k) * 44 + ci:(layer * 3 + k) * 44 + ci + 1]
                cwv = lambda k: convw.t[:, (layer * 3 + k) * 44:(layer * 3 + k) * 44 + 44]

                def load_up(jj):
                    wt, wsb = wnext()
                    wload(wt[:, :, 0:256], wup[:, 256 * jj:256 * jj + 256].rearrange("(k p) n -> p k n", p=128), [wsb[0]])
                    wload(wt[:, :, 256:512], wup[:, DFF + 256 * jj:DFF + 256 * jj + 256].rearrange("(k p) n -> p k n", p=128), [wsb[1]])
                    return wt, wsb

                def load_dn(half, kg):
                    nk = min(8, 22 - 8 * kg)
                    wt, wsb = wnext()
                    wload(wt[:, 0:nk, :], wdn[kg * 1024:kg * 1024 + nk * 128, half * 512:(half + 1) * 512].rearrange("(k p) n -> p k n", p=128), wsb)
                    return wt, wsb, nk

                for st in range(NST):
                    ts = slice(st * ST, (st + 1) * ST)
                    norm(st, 32 + layer * 8)
                    cprev = carry[(st + 1) % 2]; cnew = carry[st % 2]
                    if st > 0:
                        op("pool", lambda e: e.tensor_tensor(tmpb.t[:], cprev.t[:, :, 1], cwv(1), ALU.mult), reads=[cprev.b, convw.b], writes=[tmpb.b])
                        op("pool", lambda e: e.tensor_tensor(bnd.t[:, :, 0], cprev.t[:, :, 0], cwv(0), ALU.mult), reads=[cprev.b, convw.b], writes=[bnd.b])
                        op("pool", lambda e: e.tensor_tensor(bnd.t[:, :, 0], bnd.t[:, :, 0], tmpb.t[:], ALU.add), reads=[bnd.b, tmpb.b], writes=[bnd.b])
                        op("pool", lambda e: e.tensor_tensor(bnd.t[:, :, 1], cprev.t[:, :, 1], cwv(0), ALU.mult), reads=[cprev.b, convw.b], writes=[bnd.b])

                    def conv(bank, ci, a):
                        op("act", lambda e: e.activation(a.t[:], P[:, bank, :], AF.Identity,
                                                         bias=convb.t[:, layer * 44 + ci:layer * 44 + ci + 1], scale=cw(2, ci)),
                           reads=[pb[bank], convw.b, convb.b], writes=[a.b])
                        if st > 0:
                            op("pool", lambda e: e.tensor_tensor(a.t[:, 0:2], a.t[:, 0:2], bnd.t[:, ci, :], ALU.add), reads=[a.b, bnd.b], writes=[a.b])
                        op("dve", lambda e: e.scalar_tensor_tensor(a.t[:, 1:ST], P[:, bank, 0:ST - 1], cw(1, ci), a.t[:, 1:ST], ALU.mult, ALU.add),
                           reads=[pb[bank], convw.b, a.b], writes=[a.b])
                        op("dve", lambda e: e.scalar_tensor_tensor(a.t[:, 2:ST], P[:, bank, 0:ST - 2], cw(0, ci), a.t[:, 2:ST], ALU.mult, ALU.add),
                           reads=[pb[bank], convw.b, a.b], writes=[a.b])
                        op("act", lambda e: e.copy(cnew.t[:, ci, :], P[:, bank, ST - 2:ST]), reads=[pb[bank]], writes=[cnew.b])

                    pend = [load_up(0), load_up(1)]
                    for jj in range(11):
                        wt, wsb = pend.pop(0)
                        if jj + 2 < 11:
                            pend.append(load_up(jj + 2))
                        for sub in range(2):
                            j = 2 * jj + sub
                            bg_, bu_ = 2 * (j % 2), 2 * (j % 2) + 1
                            mm_fm(bg_, wt, [wsb[0]], sub * 128, hn, lambda k: hn.t[:, k, :])
                            mm_fm(bu_, wt, [wsb[1]], 256 + sub * 128, hn, lambda k: hn.t[:, k, :])
                            a_g = ag[j % 2]; a_u = au[j % 2]
                            conv(bg_, j, a_g)
                            conv(bu_, 22 + j, a_u)
                            op("act", lambda e: e.activation(a_g.t[:], a_g.t[:], AF.Silu), reads=[a_g.b], writes=[a_g.b])
                            op("pool", lambda e: e.tensor_tensor(g.t[:, j, :], a_g.t[:], a_u.t[:], ALU.mult), reads=[a_g.b, a_u.b], writes=[g.b])
                    seqd = [(h_, k_) for h_ in range(2) for k_ in range(3)]
                    pend = [load_dn(*seqd[0]), load_dn(*seqd[1])]
                    for idx, (half, kg) in enumerate(seqd):
                        wt, wsb, nk = pend.pop(0)
                        if idx + 2 < len(seqd):
                            pend.append(load_dn(*seqd[idx + 2]))
                        for dq in range(4):
                            for kk in range(nk):
                                op("pe", lambda e: e.matmul(P[:, 4 + dq, :], wt[:, kk, dq * 128:(dq + 1) * 128], g.t[:, 8 * kg + kk, :],
                                                            start=(kg == 0 and kk == 0), stop=(kg == 2 and kk == nk - 1)),
                                   reads=list(wsb) + [g.b], writes=[pb[4 + dq]], pe_accum=True)
                        if kg == 2:
                            for dq in range(4):
                                dc = 4 * half + dq
                                op("dve", lambda e: e.tensor_tensor(hT[:, dc, ts], P[:, 4 + dq, :], hT[:, dc, ts], ALU.add),
                                   reads=[pb[4 + dq], bh[st]], writes=[bh[st]])
                c.release([g.b, bnd.b, tmpb.b] + [t.b for t in ag + au + carry])

        def hgrn_phase(layer):
            j = layer // 2
            win = hgrn_w_in[j]
            with ExitStack() as ps_:
                qtil = sb(ps_, "qtil", [128, 8, ST], BF16); ktil = sb(ps_, "ktil", [128, 8, ST], BF16)
                khat = sb(ps_, "khat", [128, 4, D], BF16); itok = sb(ps_, "itok", [128, 4, D], BF16)
                sgg = sb(ps_, "sgg", [128, 4, D], BF16)
                E2 = [sb(ps_, f"E{i}", [128, ST]) for i in range(2)]
                elast = sb(ps_, "elast", [128, 8, 8])
                tsig = sb(ps_, "tsig", [128, ST]); tlf = sb(ps_, "tlf", [128, ST]); tk = sb(ps_, "tk", [128, ST])
                tb = sb(ps_, "tb", [128, ST]); tei = sb(ps_, "tei", [128, ST]); tnb = sb(ps_, "tnb", [128, ST])
                tsg = sb(ps_, "tsg", [128, ST])
                S = [sb(ps_, f"S{i}", [128, 8, 128]) for i in range(2)]
                Sbf = [sb(ps_, f"Sbf{i}", [128, 8, 128], BF16) for i in range(2)]
                ATm = [sb(ps_, f"ATm{i}", [128, 8, 128], BF16) for i in range(2)]
                ss = sb(ps_, "ss", [128, 8]); yg = sb(ps_, "yg", [128, D])
                oT = hn
                allb = [t.b for t in [qtil, ktil, khat, itok, sgg, elast, tsig, tlf, tk, tb, tei, tnb, tsg, ss, yg] + E2 + S + Sbf + ATm]
                for s_ in S:
                    op("dve", lambda e: e.memset(s_.t[:], 0.0), writes=[s_.b])
                for s_ in Sbf:
                    op("dve", lambda e: e.memset(s_.t[:], 0.0), writes=[s_.b])
                pbank = {"i": 0}

                def nb_():
                    pbank["i"] ^= 1
                    return pbank["i"]

                def lw(col0):
                    wt, wsb = wnext()
                    wload(wt[:], win[:, col0:col0 + 512].rearrange("(k p) n -> p k n", p=128), wsb)
                    return wt, wsb

                cg = 0
                for st in range(NST):
                    ts = slice(st * ST, (st + 1) * ST)
                    norm(st, layer * 8)
                    order = [1024, 0, 1536, 512, 2048, 2560, 3072, 3584]
                    pend = [lw(order[0])]
                    nxt = [1]

                    def take():
                        r = pend.pop(0)
                        if nxt[0] < len(order):
                            pend.append(lw(order[nxt[0]]))
                            nxt[0] += 1
                        return r

                    for grp in range(2):
                        wf, wfb = take()
                        wq, wqb = take()
                        for hh in range(4):
                            h = 4 * grp + hh
                            Eh = E2[h % 2]
                            bk = nb_()
                            mm_fm(bk, wf, wfb, hh * 128, hn, lambda k: hn.t[:, k, :])
                            op("act", lambda e: e.activation(tsig.t[:], P[:, bk, :], AF.Sigmoid), reads=[pb[bk]], writes=[tsig.b])
                            op("act", lambda e: e.activation(tlf.t[:], tsig.t[:], AF.Ln, bias=lbt.t[:, j, h:h + 1], scale=omlt.t[:, j, h:h + 1]),
                               reads=[tsig.b, lbt.b, omlt.b], writes=[tlf.b])
                            op("dve", lambda e: e.tensor_scalar(tk.t[:], tsig.t[:], nomlt.t[:, j, h:h + 1], omlt.t[:, j, h:h + 1], ALU.mult, ALU.add),
                               reads=[tsig.b, nomlt.b, omlt.b], writes=[tk.b])
                            op("dve", lambda e: e.tensor_tensor_scan(tb.t[:], scanmask.t[:], tlf.t[:], 0.0, ALU.mult, ALU.add),
                               reads=[scanmask.b, tlf.b], writes=[tb.b])
                            op("act", lambda e: e.activation(Eh.t[:], tb.t[:], AF.Exp), reads=[tb.b], writes=[Eh.b])
                            op("pool", lambda e: e.tensor_scalar(tei.t[:], tb.t[:], -80.0, None, ALU.max), reads=[tb.b], writes=[tei.b])
                            op("act", lambda e: e.activation(tei.t[:], tei.t[:], AF.Exp, scale=-1.0), reads=[tei.b], writes=[tei.b])
                            b3 = tb.t[:].rearrange("p (n c) -> p n c", c=64)
                            op("pool", lambda e: e.tensor_tensor(tnb.t[:].rearrange("p (n c) -> p n c", c=64),
                                                                 b3[:, :, 63:64].to_broadcast([128, 8, 64]), b3, ALU.subtract),
                               reads=[tb.b], writes=[tnb.b])
                            op("act", lambda e: e.activation(tnb.t[:], tnb.t[:], AF.Exp), reads=[tnb.b], writes=[tnb.b])
                            op("pool", lambda e: e.tensor_copy(elast.t[:, h, :], Eh.t[:].rearrange("p (n c) -> p n c", c=64)[:, :, 63]),
                               reads=[Eh.b], writes=[elast.b])
                            op("pool", lambda e: e.tensor_tensor(ktil.t[:, h, :], tk.t[:], tei.t[:], ALU.mult), reads=[tk.b, tei.b], writes=[ktil.b])
                            op("pool", lambda e: e.tensor_tensor(tnb.t[:], tk.t[:], tnb.t[:], ALU.mult), reads=[tk.b, tnb.b], writes=[tnb.b])
                            for n in range(4):
                                op("pe", lambda e: e.transpose(P[:, 2, n * 128:(n + 1) * 128], tnb.t[:, n * 128:(n + 1) * 128], ident.t[:]),
                                   reads=[tnb.b, ident.b], writes=[pb[2]], pe_accum=True)
                            op("act", lambda e: e.copy(khat.t[:, :, h * 128:(h + 1) * 128], P[:, 2, :].rearrange("p (n d) -> p n d", d=128)),
                               reads=[pb[2]], writes=[khat.b])
                            bk = nb_()
                            mm_fm(bk, wq, wqb, hh * 128, hn, lambda k: hn.t[:, k, :])
                            op("dve", lambda e: e.tensor_tensor(qtil.t[:, h, :], P[:, bk, :], Eh.t[:], ALU.mult), reads=[pb[bk], Eh.b], writes=[qtil.b])
                    for kind in range(2):
                        for grp in range(2):
                            wt, wtb = take()
                            for n in range(4):
                                bk = nb_()
                                for k in range(8):
                                    op("pe", lambda e: e.matmul(P[:, bk, :], hn.t[:, k, n * 128:(n + 1) * 128], wt[:, k, :], start=(k == 0), stop=(k == 7)),
                                       reads=[hn.b] + list(wtb), writes=[pb[bk]], pe_accum=True)
                                if kind == 0:
                                    op("act", lambda e: e.copy(itok.t[:, n, grp * 512:(grp + 1) * 512], P[:, bk, :]), reads=[pb[bk]], writes=[itok.b])
                                else:
                                    op("act", lambda e: e.activation(tsg.t[:], P[:, bk, :], AF.Silu), reads=[pb[bk]], writes=[tsg.b])
                                    op("pool", lambda e: e.tensor_tensor(sgg.t[:, n, grp * 512:(grp + 1) * 512].rearrange("p (h v) -> p h v", v=128),
                                                                         tsg.t[:].rearrange("p (h v) -> p h v", v=128),
                                                                         gnb.t[:, j, :].unsqueeze(1).to_broadcast([128, 4, 128]), ALU.mult),
                                       reads=[tsg.b, gnb.b], writes=[sgg.b])
                    for n in range(4):
                        am = ATm[n % 2]
                        for h in range(8):
                            op("pe", lambda e: e.matmul(P[:, 2 + h // 4, (h % 4) * 128:(h % 4 + 1) * 128], ktil.t[:, h, n * 128:(n + 1) * 128],
                                                        qtil.t[:, h, n * 128:(n + 1) * 128], start=(h % 4 == 0), stop=True, skip_group_check=True),
                               reads=[ktil.b, qtil.b], writes=[pb[2 + h // 4]], pe_accum=True)
                        op("dve", lambda e: e.tensor_tensor(am.t[:], P[:, 2:4, :].rearrange("p b (h t) -> p (b h) t", t=128),
                                                            maskbd.t[:].unsqueeze(1).to_broadcast([128, 8, 128]), ALU.mult),
                           reads=[pb[2], pb[3], maskbd.b], writes=[am.b])
                        for jc in range(2):
                            rows = slice(64 * jc, 64 * jc + 64)
                            col0 = n * 128 + 64 * jc
                            vb = (6, 7) if cg % 2 == 0 else (0, 1)
                            for h in range(8):
                                op("pe", lambda e: e.matmul(P[:, vb[h // 4], (h % 4) * 128:(h % 4 + 1) * 128], khat.t[rows, n, h * 128:(h + 1) * 128],
                                                            itok.t[rows, n, h * 128:(h + 1) * 128], start=(h % 4 == 0), stop=True, skip_group_check=True),
                                   reads=[khat.b, itok.b], writes=[pb[vb[h // 4]]], pe_accum=True)
                            sprev = Sbf[(cg + 1) % 2]
                            for h in range(8):
                                ob = 4 + h // 4
                                oc = slice((h % 4) * 128, (h % 4 + 1) * 128)
                                op("pe", lambda e: e.matmul(P[rows, ob, oc], am.t[rows, h, 64 * jc:64 * jc + 64], itok.t[rows, n, h * 128:(h + 1) * 128],
                                                            start=True, stop=(cg == 0), skip_group_check=True),
                                   reads=[am.b, itok.b], writes=[pb[ob]], pe_accum=True)
                                if cg > 0:
                                    op("pe", lambda e: e.matmul(P[rows, ob, oc], qtil.t[:, h, col0:col0 + 64], sprev.t[:, h, :],
                                                                start=False, stop=True, skip_group_check=True),
                                       reads=[qtil.b, sprev.b], writes=[pb[ob]], pe_accum=True)
                            scur = S[cg % 2]; sold = S[(cg + 1) % 2]
                            for h in range(8):
                                op("dve", lambda e: e.scalar_tensor_tensor(scur.t[:, h, :], sold.t[:, h, :], elast.t[:, h, 2 * n + jc:2 * n + jc + 1],
                                                                           P[:, vb[h // 4], (h % 4) * 128:(h % 4 + 1) * 128], ALU.mult, ALU.add),
                                   reads=[sold.b, elast.b, pb[vb[h // 4]]], writes=[scur.b])
                            op("act", lambda e: e.copy(Sbf[cg % 2].t[:], scur.t[:]), reads=[scur.b], writes=[Sbf[cg % 2].b])
                            cg += 1
                        o3 = P[:, 4:6, :].rearrange("p b (h v) -> p (b h) v", v=128)
                        op("act", lambda e: e.activation(yg.t[:].rearrange("p (h v) -> p h v", v=128), o3, AF.Square), reads=[pb[4], pb[5]], writes=[yg.b])
                        op("dve", lambda e: e.tensor_reduce(ss.t[:], yg.t[:].rearrange("p (h v) -> p h v", v=128), AX.X, ALU.add), reads=[yg.b], writes=[ss.b])
                        op("act", lambda e: e.activation(ss.t[:], ss.t[:], AF.Sqrt, bias=EPS, scale=1.0 / 128), reads=[ss.b], writes=[ss.b])
                        op("dve", lambda e: e.reciprocal(ss.t[:], ss.t[:]), reads=[ss.b], writes=[ss.b])
                        for h in range(8):
                            op("dve", lambda e: e.scalar_tensor_tensor(yg.t[:, h * 128:(h + 1) * 128], P[:, 4 + h // 4, (h % 4) * 128:(h % 4 + 1) * 128],
                                                                       ss.t[:, h:h + 1], sgg.t[:, n, h * 128:(h + 1) * 128], ALU.mult, ALU.mult),
                               reads=[pb[4 + h // 4], ss.b, sgg.b], writes=[yg.b])
                        for dc in range(8):
                            op("pe", lambda e: e.transpose(P[:, 2 + dc // 4, (dc % 4) * 128:(dc % 4 + 1) * 128], yg.t[:, dc * 128:(dc + 1) * 128], ident.t[:]),
                               reads=[yg.b, ident.b], writes=[pb[2 + dc // 4]], pe_accum=True)
                        op("act", lambda e: e.copy(oT.t[:, :, n * 128:(n + 1) * 128], P[:, 2:4, :].rearrange("p b (h t) -> p (b h) t", t=128)),
                           reads=[pb[2], pb[3]], writes=[oT.b])
                    out_proj(hgrn_w_out[j], oT, st)
                c.release(allb)

        def dsa_phase(layer):
            jd = layer // 2
            win = dsa_w_in[jd]
            wv = lambda c0, w_: win[:, c0:c0 + w_].rearrange("(k p) n -> p k n", p=128)
            with ExitStack() as ps_:
                Yp = sb(ps_, "Yp", [128, 16, 2, 128], BF16)
                kdup = sb(ps_, "kdup", [128, 2, L], BF16); kidx = sb(ps_, "kidx", [128, L], BF16)
                vaug = sb(ps_, "vaug", [128, 16, 2, 65], BF16)
                wsm = sb(ps_, "wsm", [128, 8, 8])
                qT = sb(ps_, "qT", [128, 8, ST], BF16)
                wpos = sb(ps_, "wpos", [128, ST]); wneg = sb(ps_, "wneg", [128, ST])
                qpos = sb(ps_, "qpos", [128, 4, ST], BF16); qneg = sb(ps_, "qneg", [128, 4, ST], BF16)
                scores = sb(ps_, "scores", [128, L]); work = sb(ps_, "work", [128, L]); m8 = sb(ps_, "m8", [128, 8])
                MBT = sb(ps_, "MBT", [128, 16, 128], BF16)
                ym = sb(ps_, "ym", [128, 8, 128])
                lg = [sb(ps_, f"lg{i}", [128, 8, 128]) for i in range(2)]
                pex = [sb(ps_, f"pex{i}", [128, 8, 128], BF16) for i in range(2)]
                rden = sb(ps_, "rden", [128, 16]); atok = sb(ps_, "atok", [128, D])
                aT = hn
                allb = [t.b for t in [Yp, kdup, kidx, vaug, wsm, qT, wpos, wneg, qpos, qneg, scores, work, m8, MBT, ym, rden, atok] + lg + pex]
                for g in range(8):
                    blk = g // 4
                    for q in range(32):
                        pt = 32 * (g % 4) + q
                        u0 = 255 - pt - 128 * blk
                        op("pe", lambda e: e.matmul(P[:, 1, q * 16:(q + 1) * 16], gr.t[:, u0:u0 + 128], rb.t[:, :],
                                                    start=(q == 0), stop=True, skip_group_check=True),
                           reads=[gr.b, rb.b], writes=[pb[1]], pe_accum=True)
                    pt0 = 32 * (g % 4)
                    op("dve", lambda e: e.tensor_tensor(Yp.t[:, :, blk, pt0:pt0 + 32],
                                                        P[:, 1, :].rearrange("p (t h) -> p h t", h=16),
                                                        cb.t[:].unsqueeze(2).to_broadcast([128, 16, 32]), ALU.subtract),
                       reads=[pb[1], cb.b], writes=[Yp.b])
                with nc.allow_non_contiguous_dma(reason="tiny w_idx column block"):
                    c.dma("sp", wsm.t[:], wv(1856, 8), writes=[wsm.b])
                op("dve", lambda e: e.memset(vaug.t[:], 1.0), writes=[vaug.b])
                pbank = {"i": 0}

                def nb_():
                    pbank["i"] = (pbank["i"] + 1) % 4
                    return pbank["i"]

                for st in range(NST):
                    ts = slice(st * ST, (st + 1) * ST)
                    norm(st, layer * 8)
                    wkv, wkvb = wnext()
                    for i_, (sc, dc_) in enumerate([(1024, 0), (1024, 64), (1088, 128), (1088, 192), (1792, 256), (1792, 320)]):
                        wload(wkv[:, :, dc_:dc_ + 64], wv(sc, 64), [wkvb[i_]])
                    wload(wkv[:, :, 384:512], wv(1152, 128), [wkvb[6], wkvb[7]])
                    for cc in range(3):
                        bk = nb_()
                        mm_fm(bk, wkv, wkvb, cc * 128, hn, lambda k: hn.t[:, k, :])
                        if cc < 2:
                            op("act", lambda e: e.copy(kdup.t[:, cc, ts], P[:, bk, :]), reads=[pb[bk]], writes=[kdup.b])
                        else:
                            op("act", lambda e: e.copy(kidx.t[:, ts], P[:, bk, :]), reads=[pb[bk]], writes=[kidx.b])
                    for n in range(4):
                        bk = nb_()
                        for k in range(8):
                            op("pe", lambda e: e.matmul(P[:, bk, 0:128], hn.t[:, k, n * 128:(n + 1) * 128], wkv[:, k, 384:512], start=(k == 0), stop=(k == 7)),
                               reads=[hn.b] + list(wkvb), writes=[pb[bk]], pe_accum=True)
                        op("act", lambda e: e.copy(vaug.t[:, 4 * st + n, :, 0:64], P[:, bk, 0:128].rearrange("p (a d) -> p a d", d=64)),
                           reads=[pb[bk]], writes=[vaug.b])
                    wtq = []
                    for col0 in (0, 512):
                        wt, wsb = wnext()
                        wload(wt[:], wv(col0, 512), wsb)
                        wtq.append((wt, wsb))
                    for half in range(2):
                        wt, wsb = wtq[half]
                        for cq in range(4):
                            bk = nb_()
                            mm_fm(bk, wt, wsb, cq * 128, hn, lambda k: hn.t[:, k, :])
                            op("act", lambda e: e.activation(qT.t[:, 4 * half + cq, :], P[:, bk, :], AF.Copy, scale=0.125), reads=[pb[bk]], writes=[qT.b])
                    ww, wwb = wnext()
                    op("dve", lambda e: e.tensor_scalar(ww[:].rearrange("p k (h r) -> p k h r", r=64),
                                                        wsm.t[:].unsqueeze(3).to_broadcast([128, 8, 8, 64]),
                                                        1.0 / math.sqrt(512.0), None, ALU.mult),
                       reads=[wsm.b], writes=wwb)
                    wqi, wqib = wnext()
                    wload(wqi[:], wv(1280, 512), wqib)
                    for cw_ in range(4):
                        bk = nb_()
                        mm_fm(bk, ww, wwb, cw_ * 128, hn, lambda k: hn.t[:, k, :])
                        op("dve", lambda e: e.tensor_scalar(wpos.t[:], P[:, bk, :], 0.0, None, ALU.max), reads=[pb[bk]], writes=[wpos.b])
                        op("dve", lambda e: e.tensor_scalar(wneg.t[:], P[:, bk, :], 0.0, None, ALU.min), reads=[pb[bk]], writes=[wneg.b])
                        bk = nb_()
                        mm_fm(bk, wqi, wqib, cw_ * 128, hn, lambda k: hn.t[:, k, :])
                        op("dve", lambda e: e.tensor_tensor(qpos.t[:, cw_, :], P[:, bk, :], wpos.t[:], ALU.mult), reads=[pb[bk], wpos.b], writes=[qpos.b])
                        op("dve", lambda e: e.tensor_tensor(qneg.t[:, cw_, :], P[:, bk, :], wneg.t[:], ALU.mult), reads=[pb[bk], wneg.b], writes=[qneg.b])
                    for n in range(4):
                        if dbg_stop <= 0:
                            continue
                        Tg = 4 * st + n
                        nk = 128 * (Tg + 1)
                        qc = slice(n * 128, (n + 1) * 128)
                        for kbk in range((nk + 511) // 512):
                            kw = min(512, nk - 512 * kbk)
                            ks = slice(512 * kbk, 512 * kbk + kw)
                            first = True
                            for ih in range(8):
                                r0 = (ih % 2) * 64
                                for sgn in range(2):
                                    qq = qpos if sgn == 0 else qneg
                                    bk = (ih % 2) * 2 + sgn
                                    op("pe", lambda e: e.matmul(P[:, bk, 0:kw], qq.t[r0:r0 + 64, ih // 2, qc], kidx.t[r0:r0 + 64, ks], start=True, stop=True),
                                       reads=[qq.b, kidx.b], writes=[pb[bk]], pe_accum=True)
                                    aop = ALU.max if sgn == 0 else ALU.min
                                    if first:
                                        op("dve", lambda e: e.tensor_scalar(scores.t[:, ks], P[:, bk, 0:kw], 0.0, None, aop), reads=[pb[bk]], writes=[scores.b])
                                        first = False
                                    else:
                                        op("dve", lambda e: e.scalar_tensor_tensor(scores.t[:, ks], P[:, bk, 0:kw], 0.0, scores.t[:, ks], aop, ALU.add),
                                           reads=[pb[bk], scores.b], writes=[scores.b])
                        op("pool", lambda e: e.tensor_tensor(scores.t[:, 128 * Tg:128 * Tg + 128], scores.t[:, 128 * Tg:128 * Tg + 128],
                                                             causal.t[:], ALU.add),
                           reads=[scores.b, causal.b], writes=[scores.b])
                        if dbg_stop <= 1:
                            continue
                        if Tg >= 2:
                            op("pool", lambda e: e.tensor_copy(work.t[:, 0:nk], scores.t[:, 0:nk]), reads=[scores.b], writes=[work.b])
                            for r_ in range(32):
                                op("dve", lambda e: e.max(m8.t[:], work.t[:, 0:nk]), reads=[work.b], writes=[m8.b])
                                if r_ < 31:
                                    op("dve", lambda e: e.match_replace(work.t[:, 0:nk], m8.t[:], work.t[:, 0:nk], NEG), reads=[m8.b, work.b], writes=[work.b])
                            op("dve", lambda e: e.tensor_scalar(work.t[:, 0:nk], scores.t[:, 0:nk], m8.t[:, 7:8], None, ALU.is_ge),
                               reads=[scores.b, m8.b], writes=[work.b])
                        else:
                            op("dve", lambda e: e.tensor_scalar(work.t[:, 0:nk], scores.t[:, 0:nk], -1.0e29, None, ALU.is_ge), reads=[scores.b], writes=[work.b])
                        if dbg_stop <= 2:
                            continue
                        kb = 0
                        while kb <= Tg:
                            cnt = min(4, Tg + 1 - kb)
                            for q_ in range(cnt):
                                op("pe", lambda e: e.transpose(P[:, 4, q_ * 128:(q_ + 1) * 128], work.t[:, (kb + q_) * 128:(kb + q_ + 1) * 128], ident.t[:]),
                                   reads=[work.b, ident.b], writes=[pb[4]], pe_accum=True)
                            op("dve", lambda e: e.tensor_scalar(MBT.t[:, kb:kb + cnt, :], P[:, 4, 0:cnt * 128].rearrange("p (a t) -> p a t", t=128),
                                                                BIG, -BIG, ALU.mult, ALU.add),
                               reads=[pb[4]], writes=[MBT.b])
                            kb += cnt
                        if dbg_stop <= 3:
                            continue
                        it = 0
                        for kb in range(Tg + 1):
                            near = kb >= Tg - 1
                            blk = Tg - kb
                            for hg in range(2):
                                banks = (0, 1) if it % 2 == 0 else (2, 3)
                                r_ = it % 2
                                it += 1
                                for hh in range(8):
                                    h = 8 * hg + hh
                                    r0 = (h % 2) * 64
                                    op("pe", lambda e: e.matmul(P[:, banks[hh % 2], (hh // 2) * 128:(hh // 2 + 1) * 128], kdup.t[r0:r0 + 64, h // 8, kb * 128:(kb + 1) * 128],
                                                                qT.t[r0:r0 + 64, h // 2, qc], start=(hh // 2 == 0), stop=True, skip_group_check=True),
                                       reads=[kdup.b, qT.b], writes=[pb[banks[hh % 2]]], pe_accum=True)
                                lsrc = P[:, banks[0]:banks[0] + 2, :].rearrange("p b (h t) -> p (b h) t", t=128)
                                mb_b = MBT.t[:, kb, :].unsqueeze(1).to_broadcast([128, 8, 128])
                                if near:
                                    op("pool", lambda e: e.tensor_tensor(ym.t[:].rearrange("p (two a) t -> p two a t", two=2),
                                                                         Yp.t[:, 8 * hg:8 * hg + 8, blk, :].rearrange("p (a two) t -> p two a t", two=2),
                                                                         MBT.t[:, kb, :].unsqueeze(1).unsqueeze(1).to_broadcast([128, 2, 4, 128]), ALU.add),
                                       reads=[Yp.b, MBT.b], writes=[ym.b])
                                    op("dve", lambda e: e.tensor_tensor(lg[r_].t[:], lsrc, ym.t[:], ALU.add),
                                       reads=[pb[banks[0]], pb[banks[1]], ym.b], writes=[lg[r_].b])
                                else:
                                    op("dve", lambda e: e.tensor_tensor(lg[r_].t[:], lsrc, mb_b, ALU.add),
                                       reads=[pb[banks[0]], pb[banks[1]], MBT.b], writes=[lg[r_].b])
                                op("act", lambda e: e.activation(pex[r_].t[:], lg[r_].t[:], AF.Exp), reads=[lg[r_].b], writes=[pex[r_].b])
                                for hh in range(8):
                                    h = 8 * hg + hh
                                    ob = 5 + h // 6
                                    oc = (h % 6) * 65
                                    op("pe", lambda e: e.matmul(P[:, ob, oc:oc + 65], pex[r_].t[:, (hh % 2) * 4 + hh // 2, :], vaug.t[:, kb, h // 8, :],
                                                                start=(kb == 0 and h % 6 == 0), stop=(kb == Tg), skip_group_check=True),
                                       reads=[pex[r_].b, vaug.b], writes=[pb[ob]], pe_accum=True)
                        if dbg_stop <= 4:
                            continue
                        for b3 in range(3):
                            nh = 6 if b3 < 2 else 4
                            o3 = P[:, 5 + b3, 0:nh * 65].rearrange("p (h d) -> p h d", d=65)
                            op("dve", lambda e: e.reciprocal(rden.t[:, 6 * b3:6 * b3 + nh], o3[:, :, 64]), reads=[pb[5 + b3]], writes=[rden.b])
                            op("dve", lambda e: e.tensor_tensor(atok.t[:, 384 * b3:384 * b3 + nh * 64].rearrange("p (h d) -> p h d", d=64), o3[:, :, 0:64],
                                                                rden.t[:, 6 * b3:6 * b3 + nh].unsqueeze(2).to_broadcast([128, nh, 64]), ALU.mult),
                               reads=[pb[5 + b3], rden.b], writes=[atok.b])
                        for dc in range(8):
                            op("pe", lambda e: e.transpose(P[:, dc // 4, (dc % 4) * 128:(dc % 4 + 1) * 128], atok.t[:, dc * 128:(dc + 1) * 128], ident.t[:]),
                               reads=[atok.b, ident.b], writes=[pb[dc // 4]], pe_accum=True)
                        op("act", lambda e: e.copy(aT.t[:, :, qc], P[:, 0:2, :].rearrange("p b (h t) -> p (b h) t", t=128)), reads=[pb[0], pb[1]], writes=[aT.b])
                    out_proj(dsa_w_out[jd], aT, st, banks=(2, 3))
                c.release(allb)

        for s in range(nseq):
            with ExitStack() as ps_:
                xs = [sb(ps_, f"xs{i}", [128, 4, D]) for i in range(2)]
                it = 0
                for st in range(NST):
                    xt = xs[st % 2]
                    c.dma("sp", xt.t[:], x[s, st * ST:(st + 1) * ST, :].rearrange("(n p) d -> p n d", p=128), writes=[xt.b])
                    for dc in range(8):
                        bk = it % 2; it += 1
                        for n in range(4):
                            op("pe", lambda e: e.transpose(P[:, bk, n * 128:(n + 1) * 128], xt.t[:, n, dc * 128:(dc + 1) * 128], ident.t[:]),
                               reads=[xt.b, ident.b], writes=[pb[bk]], pe_accum=True)
                        if dc % 2 == 0:
                            op("act", lambda e: e.copy(hT[:, dc, st * ST:(st + 1) * ST], P[:, bk, :]), reads=[pb[bk]], writes=[bh[st]])
                        else:
                            op("dve", lambda e: e.tensor_copy(hT[:, dc, st * ST:(st + 1) * ST], P[:, bk, :]), reads=[pb[bk]], writes=[bh[st]])
                c.release([t.b for t in xs])
            for kind_, layer in plan:
                {"hgrn": hgrn_phase, "dsa": dsa_phase, "ffn": ffn_phase}[kind_](layer)
            with ExitStack() as ps_:
                yf = sb(ps_, "yf", [128, 8, ST]); otok = [sb(ps_, f"otok{i}", [128, D]) for i in range(2)]
                it = 0
                for st in range(NST):
                    ts = slice(st * ST, (st + 1) * ST)
                    for dc in range(8):
                        s_ = sq[dc % 2]
                        op("act", lambda e: e.activation(s_.t[:], hT[:, dc, ts], AF.Square), reads=[bh[st]], writes=[s_.b])
                        op("pe", lambda e: e.matmul(P[:, 3, :], onesb.t[:], s_.t[:], start=(dc == 0), stop=(dc == 7)),
                           reads=[onesb.b, s_.b], writes=[pb[3]], pe_accum=True)
                    op("act", lambda e: e.activation(rstd.t[:], P[:, 3, :], AF.Sqrt, bias=EPS, scale=1.0 / D), reads=[pb[3]], writes=[rstd.b])
                    op("dve", lambda e: e.reciprocal(rstd.t[:], rstd.t[:]), reads=[rstd.b], writes=[rstd.b])
                    for dc in range(8):
                        op("dve", lambda e: e.scalar_tensor_tensor(yf.t[:, dc, :], hT[:, dc, ts], vec.t[:, 64 + dc:65 + dc], rstd.t[:], ALU.mult, ALU.mult),
                           reads=[bh[st], vec.b, rstd.b], writes=[yf.b])
                    for n in range(4):
                        ot = otok[it % 2]; it += 1
                        for dc in range(8):
                            op("pe", lambda e: e.transpose(P[:, dc // 4, (dc % 4) * 128:(dc % 4 + 1) * 128], yf.t[:, dc, n * 128:(n + 1) * 128], ident.t[:]),
                               reads=[yf.b, ident.b], writes=[pb[dc // 4]], pe_accum=True)
                        op("act", lambda e: e.copy(ot.t[:, 0:512], P[:, 0, :]), reads=[pb[0]], writes=[ot.b])
                        op("dve", lambda e: e.tensor_copy(ot.t[:, 512:1024], P[:, 1, :]), reads=[pb[1]], writes=[ot.b])
                        r0 = st * ST + n * 128
                        c.dma("sp", out[s, r0:r0 + 128, :], ot.t[:], reads=[ot.b])
                c.release([yf.b] + [t.b for t in otok])
        for k in range(len(c.dsem)):
            if c.dval[k] > 0:
                c._wait("sp", ("d", k, c.dval[k]))
        for eng in ("pe", "act", "dve", "pool"):
            if c.cnt[eng] > 0:
                c._wait("sp", ("e", eng, c.cnt[eng]))
        print("instr counts", c.cnt, "waits", c.nwaits, c.wstat)
    return nc


_NC_CACHE = {}


def kernel(**inputs):
    n = 8
    if "nc" not in _NC_CACHE:
        _NC_CACHE["nc"] = build_nc()
    nc = _NC_CACHE["nc"]
    consts = _host_consts()
    x = np.ascontiguousarray(np.asarray(inputs["x"], dtype=np.float32))
    shared = {k: np.ascontiguousarray(np.asarray(v, dtype=np.float32)) for k, v in inputs.items() if k != "x"}
    in_maps = []
    for i in range(n):
        m = dict(shared)
        m.update(consts)
        m["x"] = x[NSEQ * i:NSEQ * (i + 1)]
        in_maps.append(m)
    res = run_bass_kernel_spmd(nc, in_maps, core_ids=list(range(n)))
    return np.concatenate([np.asarray(r["out"]) for r in res.results], axis=0).astype(np.float32)
```

```python
import math
from contextlib import ExitStack
import numpy as np
import concourse.bass as bass
import concourse.mybir as mybir
from concourse.bass_utils import run_bass_kernel_spmd

F32 = mybir.dt.float32
BF16 = mybir.dt.bfloat16
AF = mybir.ActivationFunctionType
ALU = mybir.AluOpType
AX = mybir.AxisListType

D = 1024
L = 2048
ST = 512
NST = L // ST
DFF = 2816
NSEQ = 2
DEPTH = 4
EPS = 1e-6
NEG = -1.0e30
BIG = 30000.0
EPOCH = 24000
import os as _os
KDBG = _os.environ.get('KDBG', '')


class Buf:
    __slots__ = ("name", "w", "rs")

    def __init__(self, name=""):
        self.name = name
        self.w = None
        self.rs = {}


class Ctx:
    ENG = ("pe", "act", "dve", "pool", "sp")

    def __init__(self, nc, es, n_dma_sems=32, n_epochs=6):
        self.nc = nc
        self.e = {"pe": nc.tensor, "act": nc.scalar, "dve": nc.vector, "pool": nc.gpsimd, "sp": nc.sync}
        self.sem = {n: [es.enter_context(nc.semaphore(f"s_{n}_{k}")) for k in range(n_epochs)] for n in self.ENG}
        self.cnt = {n: 0 for n in self.ENG}
        self.seen = {n: {} for n in self.ENG}
        self.dsem = [es.enter_context(nc.semaphore(f"s_dma_{k}")) for k in range(n_dma_sems)]
        self.dval = [0] * n_dma_sems
        self.dnext = 0
        self.nwaits = 0
        self.wstat = {}

    def _wait(self, eng, tok):
        if tok is None:
            return
        if tok[0] == "e":
            _, src, idx = tok
            ep, v = divmod(idx - 1, EPOCH)
            v += 1
            seen = self.seen[eng]
            for kk in seen:
                if len(kk) == 3 and kk[1] == src and kk[2] > ep:
                    return
            key = ("e", src, ep)
            if seen.get(key, 0) >= v:
                return
            self.e[eng].wait_ge(self.sem[src][ep], v)
            seen[key] = v
            self.nwaits += 1
            self.wstat[(src, eng)] = self.wstat.get((src, eng), 0) + 1
        else:
            _, k, val = tok
            key = ("d", k)
            if self.seen[eng].get(key, 0) >= val:
                return
            self.e[eng].wait_ge(self.dsem[k], val)
            self.seen[eng][key] = val
            self.nwaits += 1

    def _deps(self, eng, reads, writes, pe_accum=False):
        for b in reads:
            self._wait(eng, b.w)
        for b in writes:
            if not (pe_accum and b.w is not None and b.w[0] == "e" and b.w[1] == "pe"):
                self._wait(eng, b.w)
            for t in b.rs.values():
                self._wait(eng, t)

    def op(self, eng, fn, reads=(), writes=(), pe_accum=False):
        self._deps(eng, reads, writes, pe_accum)
        ins = fn(self.e[eng])
        self.cnt[eng] += 1
        idx = self.cnt[eng]
        ins.then_inc(self.sem[eng][(idx - 1) // EPOCH], 1)
        tok = ("e", eng, idx)
        for b in writes:
            b.w = tok
            b.rs = {}
        for b in reads:
            b.rs[eng] = tok
        return ins

    def dma(self, eng, out, in_, reads=(), writes=(), **kw):
        k = self.dnext
        self.dnext = (self.dnext + 1) % len(self.dsem)
        if self.dval[k] > 0:
            self._wait(eng, ("d", k, self.dval[k]))
        self._deps(eng, reads, writes)
        self.dval[k] += 16
        ins = self.e[eng].dma_start(out=out, in_=in_, **kw)
        ins.then_inc(self.dsem[k], 16)
        tok = ("d", k, self.dval[k])
        for b in writes:
            b.w = tok
            b.rs = {}
        for b in reads:
            b.rs[("d", k)] = tok
        return tok

    def release(self, bufs):
        for eng in self.ENG:
            for b in bufs:
                self._wait(eng, b.w)
                for t in b.rs.values():
                    self._wait(eng, t)


class Tl:
    def __init__(self, t, name=""):
        self.t = t
        self.b = Buf(name)


def _t5_bucket(n):
    n = max(int(n), 0)
    if n < 16:
        return n
    nf = np.float32(max(n, 16))
    v = np.float32(np.log(nf / np.float32(16))) / np.float32(math.log(128 / 16)) * np.float32(16)
    return min(16 + int(v), 31)


def _host_consts():
    ident = np.eye(128, dtype=np.float32)
    maskbd = np.zeros((128, 128), np.float32)
    for s in range(128):
        for t in range(128):
            if s // 64 == t // 64 and s <= t:
                maskbd[s, t] = 1.0
    scanmask = np.ones((128, 512), np.float32)
    scanmask[:, ::64] = 0.0
    gr = np.zeros((32, 383), np.float32)
    for u in range(383):
        gr[_t5_bucket(255 - u), u] = 1.0
    causal = np.zeros((128, 128), np.float32)
    for t in range(128):
        causal[t, t + 1:] = NEG
    return {"c_ident": ident, "c_maskbd": maskbd, "c_scanmask": scanmask, "c_gr": gr, "c_causal": causal}


def build_nc(depth=DEPTH, nseq=NSEQ, plan=None, tiny=(), dbg_stop=99):
    if plan is None:
        plan = []
        for layer in range(depth):
            plan.append(("hgrn" if layer % 2 == 0 else "dsa", layer))
            plan.append(("ffn", layer))
    nc = bass.Bass("TRN2", target_bir_lowering=False)
    dt = lambda name, shape, kind="ExternalInput": nc.dram_tensor(name, [1, 1, 1] if name in tiny else list(shape), F32, kind=kind).ap()
    x = dt("x", [NSEQ, L, D])
    attn_norm = dt("attn_norm", [4, D]); ffn_norm = dt("ffn_norm", [4, D])
    hgrn_w_in = dt("hgrn_w_in", [2, D, 4 * D]); hgrn_w_out = dt("hgrn_w_out", [2, D, D])
    hgrn_gate_norm = dt("hgrn_gate_norm", [2, 128]); hgrn_lb = dt("hgrn_lower_bounds", [2, D])
    dsa_w_in = dt("dsa_w_in", [2, D, 1864]); dsa_w_out = dt("dsa_w_out", [2, D, D])
    rel_bias = dt("rel_bias", [32, 16])
    ffn_w_up = dt("ffn_w_up", [4, D, 2 * DFF]); ffn_conv_w = dt("ffn_conv_w", [4, 3, 2 * DFF])
    ffn_conv_b = dt("ffn_conv_b", [4, 2 * DFF]); ffn_w_down = dt("ffn_w_down", [4, DFF, D])
    final_norm = dt("final_norm", [D])
    c_ident = dt("c_ident", [128, 128]); c_maskbd = dt("c_maskbd", [128, 128])
    c_scanmask = dt("c_scanmask", [128, 512]); c_gr = dt("c_gr", [32, 383]); c_causal = dt("c_causal", [128, 128])
    out = dt("out", [NSEQ, L, D], kind="ExternalOutput")

    with ExitStack() as es:
        c = Ctx(nc, es)
        op = c.op

        uniq = [0]

        def sb(stack, name, shape, dtype=F32):
            uniq[0] += 1
            name = f"{name}_{uniq[0]}"
            return Tl(stack.enter_context(nc.sbuf_tensor(name, list(shape), dtype)), name)

        P = es.enter_context(nc.psum_tensor("P", [128, 8, 512], F32))
        pb = [Buf(f"pb{i}") for i in range(8)]

        hT = es.enter_context(nc.sbuf_tensor("hT", [128, 8, L], F32))
        bh = [Buf(f"h{st}") for st in range(NST)]
        ident = sb(es, "ident", [128, 128]); onesb = sb(es, "onesb", [128, 128], BF16)
        maskbd = sb(es, "maskbd", [128, 128]); scanmask = sb(es, "scanmask", [128, 512])
        gr = sb(es, "gr", [32, 383]); rb = sb(es, "rb", [32, 16]); cb = sb(es, "cb", [128, 16])
        causal = sb(es, "causal", [128, 128])
        vec = sb(es, "vec", [128, 88])
        convw = sb(es, "convw", [128, 528]); convb = sb(es, "convb", [128, 176])
        gnb = sb(es, "gnb", [128, 2, 128])
        lbt = sb(es, "lbt", [128, 2, 8]); omlt = sb(es, "omlt", [128, 2, 8]); nomlt = sb(es, "nomlt", [128, 2, 8])
        hn = sb(es, "hn", [128, 8, ST], BF16)
        sq = [sb(es, f"sq{i}", [128, ST], BF16) for i in range(2)]
        rstd = sb(es, "rstd", [128, ST])
        wbufs = [sb(es, f"wbuf{i}", [128, 8, 512], BF16) for i in range(3)]
        wsub = [[Buf(f"w{i}_{k}") for k in range(8)] for i in range(3)]
        wstate = {"i": 0}

        def wnext():
            i = wstate["i"]
            wstate["i"] = (i + 1) % 3
            return wbufs[i].t, wsub[i]

        def wload(dst, dram_ap, bufs):
            c.dma("pool", dst, dram_ap, writes=bufs)

        c.dma("sp", ident.t[:], c_ident, writes=[ident.b])
        c.dma("sp", maskbd.t[:], c_maskbd, writes=[maskbd.b])
        c.dma("sp", scanmask.t[:], c_scanmask, writes=[scanmask.b])
        c.dma("sp", gr.t[:], c_gr, writes=[gr.b])
        c.dma("sp", causal.t[:], c_causal, writes=[causal.b])
        c.dma("sp", rb.t[:], rel_bias, writes=[rb.b])
        c.dma("sp", cb.t[:], rel_bias[31:32, :].partition_broadcast(128) if False else rel_bias[31:32, :].to_broadcast([128, 16]), writes=[cb.b])
        c.dma("sp", gnb.t[:], hgrn_gate_norm.unsqueeze(0).to_broadcast([128, 2, 128]), writes=[gnb.b])
        op("dve", lambda e: e.memset(onesb.t[:], 1.0), writes=[onesb.b])

        with ExitStack() as ps_:
            stg = sb(ps_, "stg", [128, 128])

            def load_vecs(rows_ap, nrows, dst_tl, col0):
                r = 0
                while r < nrows:
                    n = min(128, nrows - r)
                    c.dma("sp", stg.t[0:n, :], rows_ap[r:r + n, :], writes=[stg.b])
                    op("pe", lambda e: e.transpose(P[:, 0, 0:n], stg.t[0:n, :], ident.t[0:n, 0:n]),
                       reads=[stg.b, ident.b], writes=[pb[0]])
                    op("dve", lambda e: e.tensor_copy(dst_tl.t[:, col0 + r:col0 + r + n], P[:, 0, 0:n]),
                       reads=[pb[0]], writes=[dst_tl.b])
                    r += n

            load_vecs(attn_norm.rearrange("l (c p) -> (l c) p", p=128), 32, vec, 0)
            load_vecs(ffn_norm.rearrange("l (c p) -> (l c) p", p=128), 32, vec, 32)
            load_vecs(final_norm.rearrange("(c p) -> c p", p=128), 8, vec, 64)
            load_vecs(hgrn_lb.rearrange("l (c p) -> (l c) p", p=128), 16, vec, 72)
            load_vecs(ffn_conv_w.rearrange("l k (c p) -> (l k c) p", p=128), 528, convw, 0)
            load_vecs(ffn_conv_b.rearrange("l (c p) -> (l c) p", p=128), 176, convb, 0)
            op("dve", lambda e: e.memset(lbt.t[:, 0, :], 0.0), writes=[lbt.b])
            op("dve", lambda e: e.tensor_tensor(lbt.t[:, 1, :], vec.t[:, 80:88], vec.t[:, 72:80], ALU.subtract),
               reads=[vec.b], writes=[lbt.b])
            op("act", lambda e: e.activation(lbt.t[:, 1, :], lbt.t[:, 1, :], AF.Sigmoid), reads=[lbt.b], writes=[lbt.b])
            op("dve", lambda e: e.tensor_scalar(omlt.t[:], lbt.t[:], -1.0, 1.0, ALU.mult, ALU.add), reads=[lbt.b], writes=[omlt.b])
            op("dve", lambda e: e.tensor_scalar(nomlt.t[:], lbt.t[:], 1.0, -1.0, ALU.mult, ALU.add), reads=[lbt.b], writes=[nomlt.b])
            c.release([stg.b])

        def norm(st, gcol0):
            ts = slice(st * ST, (st + 1) * ST)
            for dc in range(8):
                s_ = sq[dc % 2]
                op("act", lambda e: e.activation(s_.t[:], hT[:, dc, ts], AF.Square), reads=[bh[st]], writes=[s_.b])
                op("pe", lambda e: e.matmul(P[:, 3, :], onesb.t[:], s_.t[:], start=(dc == 0), stop=(dc == 7)),
                   reads=[onesb.b, s_.b], writes=[pb[3]], pe_accum=True)
            op("act", lambda e: e.activation(rstd.t[:], P[:, 3, :], AF.Sqrt, bias=EPS, scale=1.0 / D), reads=[pb[3]], writes=[rstd.b])
            op("dve", lambda e: e.reciprocal(rstd.t[:], rstd.t[:]), reads=[rstd.b], writes=[rstd.b])
            for dc in range(8):
                op("dve", lambda e: e.scalar_tensor_tensor(hn.t[:, dc, :], hT[:, dc, ts], vec.t[:, gcol0 + dc:gcol0 + dc + 1],
                                                           rstd.t[:], ALU.mult, ALU.mult),
                   reads=[bh[st], vec.b, rstd.b], writes=[hn.b])

        def mm_fm(bank, wt, wb_, col0, rhs_tl, rhs_ap_fn, nk=8):
            for k in range(nk):
                op("pe", lambda e: e.matmul(P[:, bank, :], wt[:, k, col0:col0 + 128], rhs_ap_fn(k), start=(k == 0), stop=(k == nk - 1)),
                   reads=list(wb_) + [rhs_tl.b], writes=[pb[bank]], pe_accum=True)

        def out_proj(w_dram, src_tl, st, banks=(0, 1)):
            ts = slice(st * ST, (st + 1) * ST)
            it = 0
            tiles = []
            for half in range(2):
                wt, wsb = wnext()
                wload(wt[:], w_dram[:, half * 512:(half + 1) * 512].rearrange("(k p) n -> p k n", p=128), wsb)
                tiles.append((wt, wsb))
            for half in range(2):
                wt, wsb = tiles[half]
                for dq in range(4):
                    dc = 4 * half + dq
                    bank = banks[it % 2]; it += 1
                    mm_fm(bank, wt, wsb, dq * 128, src_tl, lambda k: src_tl.t[:, k, :])
                    op("dve", lambda e: e.tensor_tensor(hT[:, dc, ts], P[:, bank, :], hT[:, dc, ts], ALU.add),
                       reads=[pb[bank], bh[st]], writes=[bh[st]])

        def ffn_phase(layer):
            with ExitStack() as ps_:
                g = sb(ps_, "g", [128, 22, ST], BF16)
                ag = [sb(ps_, f"ag{i}", [128, ST]) for i in range(2)]
                au = [sb(ps_, f"au{i}", [128, ST]) for i in range(2)]
                carry = [sb(ps_, f"carry{i}", [128, 44, 2]) for i in range(2)]
                bnd = sb(ps_, "bnd", [128, 44, 2]); tmpb = sb(ps_, "tmpb", [128, 44])
                wup = ffn_w_up[layer]
                wdn = ffn_w_down[layer]
                cw = lambda k, ci: convw.t[:, (layer * 3 + k) * 44 + ci:(layer * 3 + k) * 44 + ci + 1]
                cwv = lambda k: convw.t[:, (layer * 3 + k) * 44:(layer * 3 + k) * 44 + 44]

                def load_up(jj):
                    wt, wsb = wnext()
                    wload(wt[:, :, 0:256], wup[:, 256 * jj:256 * jj + 256].rearrange("(k p) n -> p k n", p=128), [wsb[0]])
                    wload(wt[:, :, 256:512], wup[:, DFF + 256 * jj:DFF + 256 * jj + 256].rearrange("(k p) n -> p k n", p=128), [wsb[1]])
                    return wt, wsb

                def load_dn(half, kg):
                    nk = min(8, 22 - 8 * kg)
                    wt, wsb = wnext()
                    wload(wt[:, 0:nk, :], wdn[kg * 1024:kg * 1024 + nk * 128, half * 512:(half + 1) * 512].rearrange("(k p) n -> p k n", p=128), wsb)
                    return wt, wsb, nk

                for st in range(NST):
                    ts = slice(st * ST, (st + 1) * ST)
                    norm(st, 32 + layer * 8)
                    cprev = carry[(st + 1) % 2]; cnew = carry[st % 2]
                    if st > 0:
                        op("pool", lambda e: e.tensor_tensor(tmpb.t[:], cprev.t[:, :, 1], cwv(1), ALU.mult), reads=[cprev.b, convw.b], writes=[tmpb.b])
                        op("pool", lambda e: e.tensor_tensor(bnd.t[:, :, 0], cprev.t[:, :, 0], cwv(0), ALU.mult), reads=[cprev.b, convw.b], writes=[bnd.b])
                        op("pool", lambda e: e.tensor_tensor(bnd.t[:, :, 0], bnd.t[:, :, 0], tmpb.t[:], ALU.add), reads=[bnd.b, tmpb.b], writes=[bnd.b])
                        op("pool", lambda e: e.tensor_tensor(bnd.t[:, :, 1], cprev.t[:, :, 1], cwv(0), ALU.mult), reads=[cprev.b, convw.b], writes=[bnd.b])

                    def conv(bank, ci, a):
                        op("act", lambda e: e.activation(a.t[:], P[:, bank, :], AF.Identity,
                                                         bias=convb.t[:, layer * 44 + ci:layer * 44 + ci + 1], scale=cw(2, ci)),
                           reads=[pb[bank], convw.b, convb.b], writes=[a.b])
                        if st > 0:
                            op("pool", lambda e: e.tensor_tensor(a.t[:, 0:2], a.t[:, 0:2], bnd.t[:, ci, :], ALU.add), reads=[a.b, bnd.b], writes=[a.b])
                        op("dve", lambda e: e.scalar_tensor_tensor(a.t[:, 1:ST], P[:, bank, 0:ST - 1], cw(1, ci), a.t[:, 1:ST], ALU.mult, ALU.add),
                           reads=[pb[bank], convw.b, a.b], writes=[a.b])
                        op("dve", lambda e: e.scalar_tensor_tensor(a.t[:, 2:ST], P[:, bank, 0:ST - 2], cw(0, ci), a.t[:, 2:ST], ALU.mult, ALU.add),
                           reads=[pb[bank], convw.b, a.b], writes=[a.b])
                        op("act", lambda e: e.copy(cnew.t[:, ci, :], P[:, bank, ST - 2:ST]), reads=[pb[bank]], writes=[cnew.b])

                    pend = [load_up(0), load_up(1)]
                    for jj in range(11):
                        wt, wsb = pend.pop(0)
                        if jj + 2 < 11:
                            pend.append(load_up(jj + 2))
                        for sub in range(2):
                            j = 2 * jj + sub
                            bg_, bu_ = 2 * (j % 2), 2 * (j % 2) + 1
                            mm_fm(bg_, wt, [wsb[0]], sub * 128, hn, lambda k: hn.t[:, k, :])
                            mm_fm(bu_, wt, [wsb[1]], 256 + sub * 128, hn, lambda k: hn.t[:, k, :])
                            a_g = ag[j % 2]; a_u = au[j % 2]
                            conv(bg_, j, a_g)
                            conv(bu_, 22 + j, a_u)
                            op("act", lambda e: e.activation(a_g.t[:], a_g.t[:], AF.Silu), reads=[a_g.b], writes=[a_g.b])
                            op("pool", lambda e: e.tensor_tensor(g.t[:, j, :], a_g.t[:], a_u.t[:], ALU.mult), reads=[a_g.b, a_u.b], writes=[g.b])
                    seqd = [(h_, k_) for h_ in range(2) for k_ in range(3)]
                    pend = [load_dn(*seqd[0]), load_dn(*seqd[1])]
                    for idx, (half, kg) in enumerate(seqd):
                        wt, wsb, nk = pend.pop(0)
                        if idx + 2 < len(seqd):
                            pend.append(load_dn(*seqd[idx + 2]))
                        for dq in range(4):
                            for kk in range(nk):
                                op("pe", lambda e: e.matmul(P[:, 4 + dq, :], wt[:, kk, dq * 128:(dq + 1) * 128], g.t[:, 8 * kg + kk, :],
                                                            start=(kg == 0 and kk == 0), stop=(kg == 2 and kk == nk - 1)),
                                   reads=list(wsb) + [g.b], writes=[pb[4 + dq]], pe_accum=True)
                        if kg == 2:
                            for dq in range(4):
                                dc = 4 * half + dq
                                op("dve", lambda e: e.tensor_tensor(hT[:, dc, ts], P[:, 4 + dq, :], hT[:, dc, ts], ALU.add),
                                   reads=[pb[4 + dq], bh[st]], writes=[bh[st]])
                c.release([g.b, bnd.b, tmpb.b] + [t.b for t in ag + au + carry])

        def hgrn_phase(layer):
            j = layer // 2
            win = hgrn_w_in[j]
            with ExitStack() as ps_:
                qtil = sb(ps_, "qtil", [128, 8, ST], BF16); ktil = sb(ps_, "ktil", [128, 8, ST], BF16)
                khat = sb(ps_, "khat", [128, 4, D], BF16); itok = sb(ps_, "itok", [128, 4, D], BF16)
                sgg = sb(ps_, "sgg", [128, 4, D], BF16)
                E2 = [sb(ps_, f"E{i}", [128, ST]) for i in range(2)]
                elast = sb(ps_, "elast", [128, 8, 8])
                tsig = sb(ps_, "tsig", [128, ST]); tlf = sb(ps_, "tlf", [128, ST]); tk = sb(ps_, "tk", [128, ST])
                tb = sb(ps_, "tb", [128, ST]); tei = sb(ps_, "tei", [128, ST]); tnb = sb(ps_, "tnb", [128, ST])
                tsg = sb(ps_, "tsg", [128, ST])
                S = [sb(ps_, f"S{i}", [128, 8, 128]) for i in range(2)]
                Sbf = [sb(ps_, f"Sbf{i}", [128, 8, 128], BF16) for i in range(2)]
                ATm = [sb(ps_, f"ATm{i}", [128, 8, 128], BF16) for i in range(2)]
                ss = sb(ps_, "ss", [128, 8]); yg = sb(ps_, "yg", [128, D])
                oT = hn
                allb = [t.b for t in [qtil, ktil, khat, itok, sgg, elast, tsig, tlf, tk, tb, tei, tnb, tsg, ss, yg] + E2 + S + Sbf + ATm]
                for s_ in S:
                    op("dve", lambda e: e.memset(s_.t[:], 0.0), writes=[s_.b])
                for s_ in Sbf:
                    op("dve", lambda e: e.memset(s_.t[:], 0.0), writes=[s_.b])
                pbank = {"i": 0}

                def nb_():
                    pbank["i"] ^= 1
                    return pbank["i"]

                def lw(col0):
                    wt, wsb = wnext()
                    wload(wt[:], win[:, col0:col0 + 512].rearrange("(k p) n -> p k n", p=128), wsb)
                    return wt, wsb

                cg = 0
                for st in range(NST):
                    ts = slice(st * ST, (st + 1) * ST)
                    norm(st, layer * 8)
                    order = [1024, 0, 1536, 512, 2048, 2560, 3072, 3584]
                    pend = [lw(order[0])]
                    nxt = [1]

                    def take():
                        r = pend.pop(0)
                        if nxt[0] < len(order):
                            pend.append(lw(order[nxt[0]]))
                            nxt[0] += 1
                        return r

                    for grp in range(2):
                        wf, wfb = take()
                        wq, wqb = take()
                        for hh in range(4):
                            h = 4 * grp + hh
                            Eh = E2[h % 2]
                            bk = nb_()
                            mm_fm(bk, wf, wfb, hh * 128, hn, lambda k: hn.t[:, k, :])
                            op("act", lambda e: e.activation(tsig.t[:], P[:, bk, :], AF.Sigmoid), reads=[pb[bk]], writes=[tsig.b])
                            op("act", lambda e: e.activation(tlf.t[:], tsig.t[:], AF.Ln, bias=lbt.t[:, j, h:h + 1], scale=omlt.t[:, j, h:h + 1]),
                               reads=[tsig.b, lbt.b, omlt.b], writes=[tlf.b])
                            op("dve", lambda e: e.tensor_scalar(tk.t[:], tsig.t[:], nomlt.t[:, j, h:h + 1], omlt.t[:, j, h:h + 1], ALU.mult, ALU.add),
                               reads=[tsig.b, nomlt.b, omlt.b], writes=[tk.b])
                            op("dve", lambda e: e.tensor_tensor_scan(tb.t[:], scanmask.t[:], tlf.t[:], 0.0, ALU.mult, ALU.add),
                               reads=[scanmask.b, tlf.b], writes=[tb.b])
                            op("act", lambda e: e.activation(Eh.t[:], tb.t[:], AF.Exp), reads=[tb.b], writes=[Eh.b])
                            op("pool", lambda e: e.tensor_scalar(tei.t[:], tb.t[:], 1.0e30, -80.0, ALU.min, ALU.max), reads=[tb.b], writes=[tei.b])
                            op("act", lambda e: e.activation(tei.t[:], tei.t[:], AF.Exp, scale=-1.0), reads=[tei.b], writes=[tei.b])
                            b3 = tb.t[:].rearrange("p (n c) -> p n c", c=64)
                            op("pool", lambda e: e.tensor_tensor(tnb.t[:].rearrange("p (n c) -> p n c", c=64),
                                                                 b3[:, :, 63:64].to_broadcast([128, 8, 64]), b3, ALU.subtract),
                               reads=[tb.b], writes=[tnb.b])
                            op("act", lambda e: e.activation(tnb.t[:], tnb.t[:], AF.Exp), reads=[tnb.b], writes=[tnb.b])
                            op("pool", lambda e: e.tensor_copy(elast.t[:, h, :], Eh.t[:].rearrange("p (n c) -> p n c", c=64)[:, :, 63]),
                               reads=[Eh.b], writes=[elast.b])
                            op("pool", lambda e: e.tensor_tensor(ktil.t[:, h, :], tk.t[:], tei.t[:], ALU.mult), reads=[tk.b, tei.b], writes=[ktil.b])
                            op("pool", lambda e: e.tensor_tensor(tnb.t[:], tk.t[:], tnb.t[:], ALU.mult), reads=[tk.b, tnb.b], writes=[tnb.b])
                            for n in range(4):
                                op("pe", lambda e: e.transpose(P[:, 2, n * 128:(n + 1) * 128], tnb.t[:, n * 128:(n + 1) * 128], ident.t[:]),
                                   reads=[tnb.b, ident.b], writes=[pb[2]], pe_accum=True)
                            op("act", lambda e: e.copy(khat.t[:, :, h * 128:(h + 1) * 128], P[:, 2, :].rearrange("p (n d) -> p n d", d=128)),
                               reads=[pb[2]], writes=[khat.b])
                            bk = nb_()
                            mm_fm(bk, wq, wqb, hh * 128, hn, lambda k: hn.t[:, k, :])
                            op("dve", lambda e: e.tensor_tensor(qtil.t[:, h, :], P[:, bk, :], Eh.t[:], ALU.mult), reads=[pb[bk], Eh.b], writes=[qtil.b])
                    for kind in range(2):
                        for grp in range(2):
                            wt, wtb = take()
                            for n in range(4):
                                bk = nb_()
                                for k in range(8):
                                    op("pe", lambda e: e.matmul(P[:, bk, :], hn.t[:, k, n * 128:(n + 1) * 128], wt[:, k, :], start=(k == 0), stop=(k == 7)),
                                       reads=[hn.b] + list(wtb), writes=[pb[bk]], pe_accum=True)
                                if kind == 0:
                                    op("act", lambda e: e.copy(itok.t[:, n, grp * 512:(grp + 1) * 512], P[:, bk, :]), reads=[pb[bk]], writes=[itok.b])
                                else:
                                    op("act", lambda e: e.activation(tsg.t[:], P[:, bk, :], AF.Silu), reads=[pb[bk]], writes=[tsg.b])
                                    op("pool", lambda e: e.tensor_tensor(sgg.t[:, n, grp * 512:(grp + 1) * 512].rearrange("p (h v) -> p h v", v=128),
                                                                         tsg.t[:].rearrange("p (h v) -> p h v", v=128),
                                                                         gnb.t[:, j, :].unsqueeze(1).to_broadcast([128, 4, 128]), ALU.mult),
                                       reads=[tsg.b, gnb.b], writes=[sgg.b])
                    for n in range(4):
                        am = ATm[n % 2]
                        for h in range(8):
                            op("pe", lambda e: e.matmul(P[:, 2 + h // 4, (h % 4) * 128:(h % 4 + 1) * 128], ktil.t[:, h, n * 128:(n + 1) * 128],
                                                        qtil.t[:, h, n * 128:(n + 1) * 128], start=(h % 4 == 0), stop=True, skip_group_check=True),
                               reads=[ktil.b, qtil.b], writes=[pb[2 + h // 4]], pe_accum=True)
                        op("dve", lambda e: e.tensor_tensor(am.t[:], P[:, 2:4, :].rearrange("p b (h t) -> p (b h) t", t=128),
                                                            maskbd.t[:].unsqueeze(1).to_broadcast([128, 8, 128]), ALU.mult),
                           reads=[pb[2], pb[3], maskbd.b], writes=[am.b])
                        for jc in range(2):
                            rows = slice(64 * jc, 64 * jc + 64)
                            col0 = n * 128 + 64 * jc
                            vb = (6, 7) if cg % 2 == 0 else (0, 1)
                            for h in range(8):
                                op("pe", lambda e: e.matmul(P[:, vb[h // 4], (h % 4) * 128:(h % 4 + 1) * 128], khat.t[rows, n, h * 128:(h + 1) * 128],
                                                            itok.t[rows, n, h * 128:(h + 1) * 128], start=(h % 4 == 0), stop=True, skip_group_check=True),
                                   reads=[khat.b, itok.b], writes=[pb[vb[h // 4]]], pe_accum=True)
                            sprev = Sbf[(cg + 1) % 2]
                            for h in range(8):
                                ob = 4 + h // 4
                                oc = slice((h % 4) * 128, (h % 4 + 1) * 128)
                                op("pe", lambda e: e.matmul(P[rows, ob, oc], am.t[rows, h, 64 * jc:64 * jc + 64], itok.t[rows, n, h * 128:(h + 1) * 128],
                                                            start=True, stop=(cg == 0), skip_group_check=True),
                                   reads=[am.b, itok.b], writes=[pb[ob]], pe_accum=True)
                                if cg > 0:
                                    op("pe", lambda e: e.matmul(P[rows, ob, oc], qtil.t[:, h, col0:col0 + 64], sprev.t[:, h, :],
                                                                start=False, stop=True, skip_group_check=True),
                                       reads=[qtil.b, sprev.b], writes=[pb[ob]], pe_accum=True)
                            scur = S[cg % 2]; sold = S[(cg + 1) % 2]
                            for h in range(8):
                                op("dve", lambda e: e.scalar_tensor_tensor(scur.t[:, h, :], sold.t[:, h, :], elast.t[:, h, 2 * n + jc:2 * n + jc + 1],
                                                                           P[:, vb[h // 4], (h % 4) * 128:(h % 4 + 1) * 128], ALU.mult, ALU.add),
                                   reads=[sold.b, elast.b, pb[vb[h // 4]]], writes=[scur.b])
                            op("act", lambda e: e.copy(Sbf[cg % 2].t[:], scur.t[:]), reads=[scur.b], writes=[Sbf[cg % 2].b])
                            cg += 1
                        o3 = P[:, 4:6, :].rearrange("p b (h v) -> p (b h) v", v=128)
                        op("act", lambda e: e.activation(yg.t[:].rearrange("p (h v) -> p h v", v=128), o3, AF.Square), reads=[pb[4], pb[5]], writes=[yg.b])
                        op("dve", lambda e: e.tensor_reduce(ss.t[:], yg.t[:].rearrange("p (h v) -> p h v", v=128), AX.X, ALU.add), reads=[yg.b], writes=[ss.b])
                        op("act", lambda e: e.activation(ss.t[:], ss.t[:], AF.Sqrt, bias=EPS, scale=1.0 / 128), reads=[ss.b], writes=[ss.b])
                        op("dve", lambda e: e.reciprocal(ss.t[:], ss.t[:]), reads=[ss.b], writes=[ss.b])
                        for h in range(8):
                            op("dve", lambda e: e.scalar_tensor_tensor(yg.t[:, h * 128:(h + 1) * 128], P[:, 4 + h // 4, (h % 4) * 128:(h % 4 + 1) * 128],
                                                                       ss.t[:, h:h + 1], sgg.t[:, n, h * 128:(h + 1) * 128], ALU.mult, ALU.mult),
                               reads=[pb[4 + h // 4], ss.b, sgg.b], writes=[yg.b])
                        for dc in range(8):
                            op("pe", lambda e: e.transpose(P[:, 2 + dc // 4, (dc % 4) * 128:(dc % 4 + 1) * 128], yg.t[:, dc * 128:(dc + 1) * 128], ident.t[:]),
                               reads=[yg.b, ident.b], writes=[pb[2 + dc // 4]], pe_accum=True)
                        op("act", lambda e: e.copy(oT.t[:, :, n * 128:(n + 1) * 128], P[:, 2:4, :].rearrange("p b (h t) -> p (b h) t", t=128)),
                           reads=[pb[2], pb[3]], writes=[oT.b])
                    out_proj(hgrn_w_out[j], oT, st)
                c.release(allb)

        def dsa_phase(layer):
            jd = layer // 2
            win = dsa_w_in[jd]
            wv = lambda c0, w_: win[:, c0:c0 + w_].rearrange("(k p) n -> p k n", p=128)
            with ExitStack() as ps_:
                Yp = sb(ps_, "Yp", [128, 16, 2, 128], BF16)
                kdup = sb(ps_, "kdup", [128, 2, L], BF16); kidx = sb(ps_, "kidx", [128, L], BF16)
                vaug = sb(ps_, "vaug", [128, 16, 2, 65], BF16)
                wsm = sb(ps_, "wsm", [128, 8, 8])
                qT = sb(ps_, "qT", [128, 8, ST], BF16)
                wpos = sb(ps_, "wpos", [128, ST]); wneg = sb(ps_, "wneg", [128, ST])
                qpos = sb(ps_, "qpos", [128, 4, ST], BF16); qneg = sb(ps_, "qneg", [128, 4, ST], BF16)
                scores = sb(ps_, "scores", [128, L]); work = sb(ps_, "work", [128, L]); m8 = sb(ps_, "m8", [128, 8])
                M01 = [sb(ps_, f"M01_{i}", [128, 16, 128], BF16) for i in range(2)]
                MBn = [sb(ps_, f"MBn_{i}", [128, 2, 128]) for i in range(2)]
                ym = sb(ps_, "ym", [128, 8, 128])
                lg = sb(ps_, "lg", [128, 8, 128])
                itc = [0]
                pex = [sb(ps_, f"pex{i}", [128, 8, 128], BF16) for i in range(2)]
                rden = sb(ps_, "rden", [128, 16]); atok = sb(ps_, "atok", [128, D])
                aT = hn
                allb = [t.b for t in [Yp, kdup, kidx, vaug, wsm, qT, wpos, wneg, qpos, qneg, scores, work, m8, ym, lg, rden, atok] + M01 + MBn + pex]
                for g in range(8):
                    blk = g // 4
                    for q in range(32):
                        pt = 32 * (g % 4) + q
                        u0 = 255 - pt - 128 * blk
                        op("pe", lambda e: e.matmul(P[:, 1, q * 16:(q + 1) * 16], gr.t[:, u0:u0 + 128], rb.t[:, :],
                                                    start=(q == 0), stop=True, skip_group_check=True),
                           reads=[gr.b, rb.b], writes=[pb[1]], pe_accum=True)
                    pt0 = 32 * (g % 4)
                    op("dve", lambda e: e.tensor_tensor(Yp.t[:, :, blk, pt0:pt0 + 32],
                                                        P[:, 1, :].rearrange("p (t h) -> p h t", h=16),
                                                        cb.t[:].unsqueeze(2).to_broadcast([128, 16, 32]), ALU.subtract),
                       reads=[pb[1], cb.b], writes=[Yp.b])
                with nc.allow_non_contiguous_dma(reason="tiny w_idx column block"):
                    c.dma("sp", wsm.t[:], wv(1856, 8), writes=[wsm.b])
                op("dve", lambda e: e.memset(vaug.t[:], 1.0), writes=[vaug.b])
                pbank = {"i": 0}

                def nb_():
                    pbank["i"] = (pbank["i"] + 1) % 4
                    return pbank["i"]

                for st in range(NST):
                    ts = slice(st * ST, (st + 1) * ST)
                    norm(st, layer * 8)
                    wkv, wkvb = wnext()
                    for i_, (sc, dc_) in enumerate([(1024, 0), (1024, 64), (1088, 128), (1088, 192), (1792, 256), (1792, 320)]):
                        wload(wkv[:, :, dc_:dc_ + 64], wv(sc, 64), [wkvb[i_]])
                    wload(wkv[:, :, 384:512], wv(1152, 128), [wkvb[6], wkvb[7]])
                    for cc in range(3):
                        bk = nb_()
                        mm_fm(bk, wkv, wkvb, cc * 128, hn, lambda k: hn.t[:, k, :])
                        if cc < 2:
                            op("act", lambda e: e.copy(kdup.t[:, cc, ts], P[:, bk, :]), reads=[pb[bk]], writes=[kdup.b])
                        else:
                            op("act", lambda e: e.copy(kidx.t[:, ts], P[:, bk, :]), reads=[pb[bk]], writes=[kidx.b])
                    for n in range(4):
                        bk = nb_()
                        for k in range(8):
                            op("pe", lambda e: e.matmul(P[:, bk, 0:128], hn.t[:, k, n * 128:(n + 1) * 128], wkv[:, k, 384:512], start=(k == 0), stop=(k == 7)),
                               reads=[hn.b] + list(wkvb), writes=[pb[bk]], pe_accum=True)
                        op("act", lambda e: e.copy(vaug.t[:, 4 * st + n, :, 0:64], P[:, bk, 0:128].rearrange("p (a d) -> p a d", d=64)),
                           reads=[pb[bk]], writes=[vaug.b])
                    wtq = []
                    for col0 in (0, 512):
                        wt, wsb = wnext()
                        wload(wt[:], wv(col0, 512), wsb)
                        wtq.append((wt, wsb))
                    for half in range(2):
                        wt, wsb = wtq[half]
                        for cq in range(4):
                            bk = nb_()
                            mm_fm(bk, wt, wsb, cq * 128, hn, lambda k: hn.t[:, k, :])
                            op("act", lambda e: e.activation(qT.t[:, 4 * half + cq, :], P[:, bk, :], AF.Copy, scale=0.125), reads=[pb[bk]], writes=[qT.b])
                    ww, wwb = wnext()
                    op("dve", lambda e: e.tensor_scalar(ww[:].rearrange("p k (h r) -> p k h r", r=64),
                                                        wsm.t[:].unsqueeze(3).to_broadcast([128, 8, 8, 64]),
                                                        1.0 / math.sqrt(512.0), None, ALU.mult),
                       reads=[wsm.b], writes=wwb)
                    wqi, wqib = wnext()
                    wload(wqi[:], wv(1280, 512), wqib)
                    for cw_ in range(4):
                        bk = nb_()
                        mm_fm(bk, ww, wwb, cw_ * 128, hn, lambda k: hn.t[:, k, :])
                        op("dve", lambda e: e.tensor_scalar(wpos.t[:], P[:, bk, :], 0.0, None, ALU.max), reads=[pb[bk]], writes=[wpos.b])
                        op("dve", lambda e: e.tensor_scalar(wneg.t[:], P[:, bk, :], 0.0, None, ALU.min), reads=[pb[bk]], writes=[wneg.b])
                        bk = nb_()
                        mm_fm(bk, wqi, wqib, cw_ * 128, hn, lambda k: hn.t[:, k, :])
                        op("dve", lambda e: e.tensor_tensor(qpos.t[:, cw_, :], P[:, bk, :], wpos.t[:], ALU.mult), reads=[pb[bk], wpos.b], writes=[qpos.b])
                        op("dve", lambda e: e.tensor_tensor(qneg.t[:, cw_, :], P[:, bk, :], wneg.t[:], ALU.mult), reads=[pb[bk], wneg.b], writes=[qneg.b])
                    def stage_A(n):
                        Tg = 4 * st + n
                        nk = 128 * (Tg + 1)
                        qc = slice(n * 128, (n + 1) * 128)
                        for kbk in range((nk + 511) // 512):
                            kw = min(512, nk - 512 * kbk)
                            ks = slice(512 * kbk, 512 * kbk + kw)
                            first = True
                            for ih in range(8):
                                r0 = (ih % 2) * 64
                                for sgn in range(2):
                                    qq = qpos if sgn == 0 else qneg
                                    bk = (ih % 2) * 2 + sgn
                                    op("pe", lambda e: e.matmul(P[:, bk, 0:kw], qq.t[r0:r0 + 64, ih // 2, qc], kidx.t[r0:r0 + 64, ks], start=True, stop=True),
                                       reads=[qq.b, kidx.b], writes=[pb[bk]], pe_accum=True)
                                    aop = ALU.max if sgn == 0 else ALU.min
                                    if first:
                                        op("dve", lambda e: e.tensor_scalar(scores.t[:, ks], P[:, bk, 0:kw], 0.0, None, aop), reads=[pb[bk]], writes=[scores.b])
                                        first = False
                                    else:
                                        op("dve", lambda e: e.scalar_tensor_tensor(scores.t[:, ks], P[:, bk, 0:kw], 0.0, scores.t[:, ks], aop, ALU.add),
                                           reads=[pb[bk], scores.b], writes=[scores.b])
                        op("pool", lambda e: e.tensor_tensor(scores.t[:, 128 * Tg:128 * Tg + 128], scores.t[:, 128 * Tg:128 * Tg + 128],
                                                             causal.t[:], ALU.add),
                           reads=[scores.b, causal.b], writes=[scores.b])

                    def stage_B(n):
                        Tg = 4 * st + n
                        nk = 128 * (Tg + 1)
                        if Tg >= 2:
                            for r_ in range(32):
                                src = scores if r_ == 0 else work
                                op("dve", lambda e: e.max(m8.t[:], src.t[:, 0:nk]), reads=[src.b], writes=[m8.b])
                                if r_ < 31:
                                    op("dve", lambda e: e.match_replace(work.t[:, 0:nk], m8.t[:], src.t[:, 0:nk], NEG), reads=[m8.b, src.b], writes=[work.b])
                            op("dve", lambda e: e.tensor_scalar(work.t[:, 0:nk], scores.t[:, 0:nk], m8.t[:, 7:8], None, ALU.is_ge),
                               reads=[scores.b, m8.b], writes=[work.b])
                        else:
                            op("dve", lambda e: e.tensor_scalar(work.t[:, 0:nk], scores.t[:, 0:nk], -1.0e29, None, ALU.is_ge), reads=[scores.b], writes=[work.b])

                    def stage_C(n):
                        Tg = 4 * st + n
                        m01 = M01[Tg % 2]; mbn = MBn[Tg % 2]
                        kb = 0
                        while kb <= Tg:
                            cnt = min(4, Tg + 1 - kb)
                            for q_ in range(cnt):
                                op("pe", lambda e: e.transpose(P[:, 4, q_ * 128:(q_ + 1) * 128], work.t[:, (kb + q_) * 128:(kb + q_ + 1) * 128], ident.t[:]),
                                   reads=[work.b, ident.b], writes=[pb[4]], pe_accum=True)
                            op("dve", lambda e: e.tensor_copy(m01.t[:, kb:kb + cnt, :], P[:, 4, 0:cnt * 128].rearrange("p (a t) -> p a t", t=128)),
                               reads=[pb[4]], writes=[m01.b])
                            for q_ in range(cnt):
                                blk = Tg - (kb + q_)
                                if blk <= 1:
                                    op("dve", lambda e: e.tensor_scalar(mbn.t[:, blk, :], P[:, 4, q_ * 128:(q_ + 1) * 128], BIG, -BIG, ALU.mult, ALU.add),
                                       reads=[pb[4]], writes=[mbn.b])
                            kb += cnt

                    def stage_D(n):
                        Tg = 4 * st + n
                        qc = slice(n * 128, (n + 1) * 128)
                        m01 = M01[Tg % 2]; mbn = MBn[Tg % 2]
                        kbs = [Tg] + ([Tg - 1] if Tg >= 1 else []) + list(range(0, Tg - 1))
                        for kb in kbs:
                            near = kb >= Tg - 1
                            blk = Tg - kb
                            for hg in range(2):
                                it = itc[0]; itc[0] += 1
                                banks = (0, 1) if it % 2 == 0 else (2, 3)
                                r_ = it % 2
                                for hh in range(8):
                                    h = 8 * hg + hh
                                    r0 = (h % 2) * 64
                                    op("pe", lambda e: e.matmul(P[:, banks[hh % 2], (hh // 2) * 128:(hh // 2 + 1) * 128], kdup.t[r0:r0 + 64, h // 8, kb * 128:(kb + 1) * 128],
                                                                qT.t[r0:r0 + 64, h // 2, qc], start=(hh // 2 == 0), stop=True, skip_group_check=True),
                                       reads=[kdup.b, qT.b], writes=[pb[banks[hh % 2]]], pe_accum=True)
                                lsrc = P[:, banks[0]:banks[0] + 2, :].rearrange("p b (h t) -> p (b h) t", t=128)
                                if near:
                                    op("pool", lambda e: e.tensor_tensor(ym.t[:].rearrange("p (two a) t -> p two a t", two=2),
                                                                         Yp.t[:, 8 * hg:8 * hg + 8, blk, :].rearrange("p (a two) t -> p two a t", two=2),
                                                                         mbn.t[:, blk, :].unsqueeze(1).unsqueeze(1).to_broadcast([128, 2, 4, 128]), ALU.add),
                                       reads=[Yp.b, mbn.b], writes=[ym.b])
                                    op("dve", lambda e: e.tensor_tensor(lg.t[:], lsrc, ym.t[:], ALU.add),
                                       reads=[pb[banks[0]], pb[banks[1]], ym.b], writes=[lg.b])
                                    op("act", lambda e: e.activation(pex[r_].t[:], lg.t[:], AF.Exp), reads=[lg.b], writes=[pex[r_].b])
                                else:
                                    op("act", lambda e: e.activation(pex[r_].t[:], lsrc, AF.Exp), reads=[pb[banks[0]], pb[banks[1]]], writes=[pex[r_].b])
                                    if KDBG != "nopoolmask":
                                        op("pool", lambda e: e.tensor_tensor(pex[r_].t[:], pex[r_].t[:], m01.t[:, kb, :].unsqueeze(1).to_broadcast([128, 8, 128]), ALU.mult),
                                           reads=[pex[r_].b, m01.b], writes=[pex[r_].b])
                                for hh in range(8):
                                    h = 8 * hg + hh
                                    ob = 5 + h // 6
                                    oc = (h % 6) * 65
                                    op("pe", lambda e: e.matmul(P[:, ob, oc:oc + 65], pex[r_].t[:, (hh % 2) * 4 + hh // 2, :], vaug.t[:, kb, h // 8, :],
                                                                start=(kb == kbs[0] and h % 6 == 0), stop=(kb == kbs[-1]), skip_group_check=True),
                                       reads=[pex[r_].b, vaug.b], writes=[pb[ob]], pe_accum=True)

                    def stage_E(n):
                        qc = slice(n * 128, (n + 1) * 128)
                        for b3 in range(3):
                            nh = 6 if b3 < 2 else 4
                            o3 = P[:, 5 + b3, 0:nh * 65].rearrange("p (h d) -> p h d", d=65)
                            op("dve", lambda e: e.reciprocal(rden.t[:, 6 * b3:6 * b3 + nh], o3[:, :, 64]), reads=[pb[5 + b3]], writes=[rden.b])
                            op("dve", lambda e: e.tensor_tensor(atok.t[:, 384 * b3:384 * b3 + nh * 64].rearrange("p (h d) -> p h d", d=64), o3[:, :, 0:64],
                                                                rden.t[:, 6 * b3:6 * b3 + nh].unsqueeze(2).to_broadcast([128, nh, 64]), ALU.mult),
                               reads=[pb[5 + b3], rden.b], writes=[atok.b])
                        for dc in range(8):
                            op("pe", lambda e: e.transpose(P[:, dc // 4, (dc % 4) * 128:(dc % 4 + 1) * 128], atok.t[:, dc * 128:(dc + 1) * 128], ident.t[:]),
                               reads=[atok.b, ident.b], writes=[pb[dc // 4]], pe_accum=True)
                        op("act", lambda e: e.copy(aT.t[:, :, qc], P[:, 0:2, :].rearrange("p b (h t) -> p (b h) t", t=128)), reads=[pb[0], pb[1]], writes=[aT.b])

                    if KDBG.startswith("seq"):
                        for n in range(4):
                            stage_A(n); stage_B(n)
                            if KDBG == "seqB":
                                continue
                            stage_C(n)
                            if KDBG == "seqC":
                                continue
                            stage_D(n); stage_E(n)
                    elif dbg_stop > 0:
                        stage_A(0); stage_B(0); stage_C(0)
                        for n in range(4):
                            if n + 1 < 4:
                                stage_A(n + 1)
                            stage_D(n)
                            if n + 1 < 4:
                                stage_B(n + 1)
                            stage_E(n)
                            if n + 1 < 4:
                                stage_C(n + 1)
                    out_proj(dsa_w_out[jd], aT, st, banks=(2, 3))
                c.release(allb)

        for s in range(nseq):
            with ExitStack() as ps_:
                xs = [sb(ps_, f"xs{i}", [128, 4, D]) for i in range(2)]
                it = 0
                for st in range(NST):
                    xt = xs[st % 2]
                    c.dma("sp", xt.t[:], x[s, st * ST:(st + 1) * ST, :].rearrange("(n p) d -> p n d", p=128), writes=[xt.b])
                    for dc in range(8):
                        bk = it % 2; it += 1
                        for n in range(4):
                            op("pe", lambda e: e.transpose(P[:, bk, n * 128:(n + 1) * 128], xt.t[:, n, dc * 128:(dc + 1) * 128], ident.t[:]),
                               reads=[xt.b, ident.b], writes=[pb[bk]], pe_accum=True)
                        if dc % 2 == 0:
                            op("act", lambda e: e.copy(hT[:, dc, st * ST:(st + 1) * ST], P[:, bk, :]), reads=[pb[bk]], writes=[bh[st]])
                        else:
                            op("dve", lambda e: e.tensor_copy(hT[:, dc, st * ST:(st + 1) * ST], P[:, bk, :]), reads=[pb[bk]], writes=[bh[st]])
                c.release([t.b for t in xs])
            for kind_, layer in plan:
                {"hgrn": hgrn_phase, "dsa": dsa_phase, "ffn": ffn_phase}[kind_](layer)
            with ExitStack() as ps_:
                yf = sb(ps_, "yf", [128, 8, ST]); otok = [sb(ps_, f"otok{i}", [128, D]) for i in range(2)]
                it = 0
                for st in range(NST):
                    ts = slice(st * ST, (st + 1) * ST)
                    for dc in range(8):
                        s_ = sq[dc % 2]
                        op("act", lambda e: e.activation(s_.t[:], hT[:, dc, ts], AF.Square), reads=[bh[st]], writes=[s_.b])
                        op("pe", lambda e: e.matmul(P[:, 3, :], onesb.t[:], s_.t[:], start=(dc == 0), stop=(dc == 7)),
                           reads=[onesb.b, s_.b], writes=[pb[3]], pe_accum=True)
                    op("act", lambda e: e.activation(rstd.t[:], P[:, 3, :], AF.Sqrt, bias=EPS, scale=1.0 / D), reads=[pb[3]], writes=[rstd.b])
                    op("dve", lambda e: e.reciprocal(rstd.t[:], rstd.t[:]), reads=[rstd.b], writes=[rstd.b])
                    for dc in range(8):
                        op("dve", lambda e: e.scalar_tensor_tensor(yf.t[:, dc, :], hT[:, dc, ts], vec.t[:, 64 + dc:65 + dc], rstd.t[:], ALU.mult, ALU.mult),
                           reads=[bh[st], vec.b, rstd.b], writes=[yf.b])
                    for n in range(4):
                        ot = otok[it % 2]; it += 1
                        for dc in range(8):
                            op("pe", lambda e: e.transpose(P[:, dc // 4, (dc % 4) * 128:(dc % 4 + 1) * 128], yf.t[:, dc, n * 128:(n + 1) * 128], ident.t[:]),
                               reads=[yf.b, ident.b], writes=[pb[dc // 4]], pe_accum=True)
                        op("act", lambda e: e.copy(ot.t[:, 0:512], P[:, 0, :]), reads=[pb[0]], writes=[ot.b])
                        op("dve", lambda e: e.tensor_copy(ot.t[:, 512:1024], P[:, 1, :]), reads=[pb[1]], writes=[ot.b])
                        r0 = st * ST + n * 128
                        c.dma("sp", out[s, r0:r0 + 128, :], ot.t[:], reads=[ot.b])
                c.release([yf.b] + [t.b for t in otok])
        for k in range(len(c.dsem)):
            if c.dval[k] > 0:
                c._wait("sp", ("d", k, c.dval[k]))
        for eng in ("pe", "act", "dve", "pool"):
            if c.cnt[eng] > 0:
                c._wait("sp", ("e", eng, c.cnt[eng]))
        print("instr counts", c.cnt, "waits", c.nwaits, c.wstat)
    return nc


_NC_CACHE = {}


def kernel(**inputs):
    n = 8
    if "nc" not in _NC_CACHE:
        _NC_CACHE["nc"] = build_nc()
    nc = _NC_CACHE["nc"]
    consts = _host_consts()
    x = np.ascontiguousarray(np.asarray(inputs["x"], dtype=np.float32))
    shared = {k: np.ascontiguousarray(np.asarray(v, dtype=np.float32)) for k, v in inputs.items() if k != "x"}
    in_maps = []
    for i in range(n):
        m = dict(shared)
        m.update(consts)
        m["x"] = x[NSEQ * i:NSEQ * (i + 1)]
        in_maps.append(m)
    res = run_bass_kernel_spmd(nc, in_maps, core_ids=list(range(n)))
    return np.concatenate([np.asarray(r["out"]) for r in res.results], axis=0).astype(np.float32)
```

```python
import math
from contextlib import ExitStack
import numpy as np
import concourse.bass as bass
import concourse.mybir as mybir
from concourse.bass_utils import run_bass_kernel_spmd

F32 = mybir.dt.float32
BF16 = mybir.dt.bfloat16
AF = mybir.ActivationFunctionType
ALU = mybir.AluOpType
AX = mybir.AxisListType

D = 1024
L = 2048
ST = 512
NST = L // ST
DFF = 2816
NSEQ = 2
DEPTH = 4
EPS = 1e-6
NEG = -1.0e30
BIG = 30000.0
EPOCH = 24000
import os as _os
KDBG = _os.environ.get('KDBG', '')


class Buf:
    __slots__ = ("name", "w", "rs")

    def __init__(self, name=""):
        self.name = name
        self.w = None
        self.rs = {}


class Ctx:
    ENG = ("pe", "act", "dve", "pool", "sp")

    def __init__(self, nc, es, n_dma_sems=32, n_epochs=6):
        self.nc = nc
        self.e = {"pe": nc.tensor, "act": nc.scalar, "dve": nc.vector, "pool": nc.gpsimd, "sp": nc.sync}
        self.sem = {n: [es.enter_context(nc.semaphore(f"s_{n}_{k}")) for k in range(n_epochs)] for n in self.ENG}
        self.cnt = {n: 0 for n in self.ENG}
        self.seen = {n: {} for n in self.ENG}
        self.dsem = [es.enter_context(nc.semaphore(f"s_dma_{k}")) for k in range(n_dma_sems)]
        self.dval = [0] * n_dma_sems
        self.dnext = 0
        self.nwaits = 0
        self.wstat = {}

    def _wait(self, eng, tok):
        if tok is None:
            return
        if tok[0] == "e":
            _, src, idx = tok
            ep, v = divmod(idx - 1, EPOCH)
            v += 1
            seen = self.seen[eng]
            for kk in seen:
                if len(kk) == 3 and kk[1] == src and kk[2] > ep:
                    return
            key = ("e", src, ep)
            if seen.get(key, 0) >= v:
                return
            self.e[eng].wait_ge(self.sem[src][ep], v)
            seen[key] = v
            self.nwaits += 1
            self.wstat[(src, eng)] = self.wstat.get((src, eng), 0) + 1
        else:
            _, k, val = tok
            key = ("d", k)
            if self.seen[eng].get(key, 0) >= val:
                return
            self.e[eng].wait_ge(self.dsem[k], val)
            self.seen[eng][key] = val
            self.nwaits += 1

    def _deps(self, eng, reads, writes, pe_accum=False):
        for b in reads:
            self._wait(eng, b.w)
        for b in writes:
            if not (pe_accum and b.w is not None and b.w[0] == "e" and b.w[1] == "pe"):
                self._wait(eng, b.w)
            for t in b.rs.values():
                self._wait(eng, t)

    def op(self, eng, fn, reads=(), writes=(), pe_accum=False):
        self._deps(eng, reads, writes, pe_accum)
        ins = fn(self.e[eng])
        self.cnt[eng] += 1
        idx = self.cnt[eng]
        ins.then_inc(self.sem[eng][(idx - 1) // EPOCH], 1)
        tok = ("e", eng, idx)
        for b in writes:
            b.w = tok
            b.rs = {}
        for b in reads:
            b.rs[eng] = tok
        return ins

    def dma(self, eng, out, in_, reads=(), writes=(), **kw):
        k = self.dnext
        self.dnext = (self.dnext + 1) % len(self.dsem)
        if self.dval[k] > 0:
            self._wait(eng, ("d", k, self.dval[k]))
        self._deps(eng, reads, writes)
        self.dval[k] += 16
        ins = self.e[eng].dma_start(out=out, in_=in_, **kw)
        ins.then_inc(self.dsem[k], 16)
        tok = ("d", k, self.dval[k])
        for b in writes:
            b.w = tok
            b.rs = {}
        for b in reads:
            b.rs[("d", k)] = tok
        return tok

    def release(self, bufs):
        for eng in self.ENG:
            for b in bufs:
                self._wait(eng, b.w)
                for t in b.rs.values():
                    self._wait(eng, t)


class Tl:
    def __init__(self, t, name=""):
        self.t = t
        self.b = Buf(name)


def _t5_bucket(n):
    n = max(int(n), 0)
    if n < 16:
        return n
    nf = np.float32(max(n, 16))
    v = np.float32(np.log(nf / np.float32(16))) / np.float32(math.log(128 / 16)) * np.float32(16)
    return min(16 + int(v), 31)


def _host_consts():
    ident = np.eye(128, dtype=np.float32)
    maskbd = np.zeros((128, 128), np.float32)
    for s in range(128):
        for t in range(128):
            if s // 64 == t // 64 and s <= t:
                maskbd[s, t] = 1.0
    scanmask = np.ones((128, 512), np.float32)
    scanmask[:, ::64] = 0.0
    gr = np.zeros((32, 383), np.float32)
    for u in range(383):
        gr[_t5_bucket(255 - u), u] = 1.0
    causal = np.zeros((128, 128), np.float32)
    for t in range(128):
        causal[t, t + 1:] = NEG
    return {"c_ident": ident, "c_maskbd": maskbd, "c_scanmask": scanmask, "c_gr": gr, "c_causal": causal}


def build_nc(depth=DEPTH, nseq=NSEQ, plan=None, tiny=(), dbg_stop=99):
    if plan is None:
        plan = []
        for layer in range(depth):
            plan.append(("hgrn" if layer % 2 == 0 else "dsa", layer))
            plan.append(("ffn", layer))
    nc = bass.Bass("TRN2", target_bir_lowering=False)
    dt = lambda name, shape, kind="ExternalInput": nc.dram_tensor(name, [1, 1, 1] if name in tiny else list(shape), F32, kind=kind).ap()
    x = dt("x", [NSEQ, L, D])
    attn_norm = dt("attn_norm", [4, D]); ffn_norm = dt("ffn_norm", [4, D])
    hgrn_w_in = dt("hgrn_w_in", [2, D, 4 * D]); hgrn_w_out = dt("hgrn_w_out", [2, D, D])
    hgrn_gate_norm = dt("hgrn_gate_norm", [2, 128]); hgrn_lb = dt("hgrn_lower_bounds", [2, D])
    dsa_w_in = dt("dsa_w_in", [2, D, 1864]); dsa_w_out = dt("dsa_w_out", [2, D, D])
    rel_bias = dt("rel_bias", [32, 16])
    ffn_w_up = dt("ffn_w_up", [4, D, 2 * DFF]); ffn_conv_w = dt("ffn_conv_w", [4, 3, 2 * DFF])
    ffn_conv_b = dt("ffn_conv_b", [4, 2 * DFF]); ffn_w_down = dt("ffn_w_down", [4, DFF, D])
    final_norm = dt("final_norm", [D])
    c_ident = dt("c_ident", [128, 128]); c_maskbd = dt("c_maskbd", [128, 128])
    c_scanmask = dt("c_scanmask", [128, 512]); c_gr = dt("c_gr", [32, 383]); c_causal = dt("c_causal", [128, 128])
    out = dt("out", [NSEQ, L, D], kind="ExternalOutput")

    with ExitStack() as es:
        c = Ctx(nc, es)
        op = c.op

        uniq = [0]

        def sb(stack, name, shape, dtype=F32):
            uniq[0] += 1
            name = f"{name}_{uniq[0]}"
            return Tl(stack.enter_context(nc.sbuf_tensor(name, list(shape), dtype)), name)

        P = es.enter_context(nc.psum_tensor("P", [128, 8, 512], F32))
        pb = [Buf(f"pb{i}") for i in range(8)]

        hT = es.enter_context(nc.sbuf_tensor("hT", [128, 8, L], F32))
        bh = [Buf(f"h{st}") for st in range(NST)]
        ident = sb(es, "ident", [128, 128]); onesb = sb(es, "onesb", [128, 128], BF16)
        maskbd = sb(es, "maskbd", [128, 128]); scanmask = sb(es, "scanmask", [128, 512])
        gr = sb(es, "gr", [32, 383]); rb = sb(es, "rb", [32, 16]); cb = sb(es, "cb", [128, 16])
        causal = sb(es, "causal", [128, 128])
        vec = sb(es, "vec", [128, 88])
        convw = sb(es, "convw", [128, 528]); convb = sb(es, "convb", [128, 176])
        gnb = sb(es, "gnb", [128, 2, 128])
        lbt = sb(es, "lbt", [128, 2, 8]); omlt = sb(es, "omlt", [128, 2, 8]); nomlt = sb(es, "nomlt", [128, 2, 8])
        hn = sb(es, "hn", [128, 8, ST], BF16)
        sq = [sb(es, f"sq{i}", [128, ST], BF16) for i in range(2)]
        rstd = sb(es, "rstd", [128, ST])
        wbufs = [sb(es, f"wbuf{i}", [128, 8, 512], BF16) for i in range(3)]
        wsub = [[Buf(f"w{i}_{k}") for k in range(8)] for i in range(3)]
        wstate = {"i": 0}

        def wnext():
            i = wstate["i"]
            wstate["i"] = (i + 1) % 3
            return wbufs[i].t, wsub[i]

        def wload(dst, dram_ap, bufs):
            c.dma("pool", dst, dram_ap, writes=bufs)

        c.dma("sp", ident.t[:], c_ident, writes=[ident.b])
        c.dma("sp", maskbd.t[:], c_maskbd, writes=[maskbd.b])
        c.dma("sp", scanmask.t[:], c_scanmask, writes=[scanmask.b])
        c.dma("sp", gr.t[:], c_gr, writes=[gr.b])
        c.dma("sp", causal.t[:], c_causal, writes=[causal.b])
        c.dma("sp", rb.t[:], rel_bias, writes=[rb.b])
        c.dma("sp", cb.t[:], rel_bias[31:32, :].partition_broadcast(128) if False else rel_bias[31:32, :].to_broadcast([128, 16]), writes=[cb.b])
        c.dma("sp", gnb.t[:], hgrn_gate_norm.unsqueeze(0).to_broadcast([128, 2, 128]), writes=[gnb.b])
        op("dve", lambda e: e.memset(onesb.t[:], 1.0), writes=[onesb.b])

        with ExitStack() as ps_:
            stg = sb(ps_, "stg", [128, 128])

            def load_vecs(rows_ap, nrows, dst_tl, col0):
                r = 0
                while r < nrows:
                    n = min(128, nrows - r)
                    c.dma("sp", stg.t[0:n, :], rows_ap[r:r + n, :], writes=[stg.b])
                    op("pe", lambda e: e.transpose(P[:, 0, 0:n], stg.t[0:n, :], ident.t[0:n, 0:n]),
                       reads=[stg.b, ident.b], writes=[pb[0]])
                    op("dve", lambda e: e.tensor_copy(dst_tl.t[:, col0 + r:col0 + r + n], P[:, 0, 0:n]),
                       reads=[pb[0]], writes=[dst_tl.b])
                    r += n

            load_vecs(attn_norm.rearrange("l (c p) -> (l c) p", p=128), 32, vec, 0)
            load_vecs(ffn_norm.rearrange("l (c p) -> (l c) p", p=128), 32, vec, 32)
            load_vecs(final_norm.rearrange("(c p) -> c p", p=128), 8, vec, 64)
            load_vecs(hgrn_lb.rearrange("l (c p) -> (l c) p", p=128), 16, vec, 72)
            load_vecs(ffn_conv_w.rearrange("l k (c p) -> (l k c) p", p=128), 528, convw, 0)
            load_vecs(ffn_conv_b.rearrange("l (c p) -> (l c) p", p=128), 176, convb, 0)
            op("dve", lambda e: e.memset(lbt.t[:, 0, :], 0.0), writes=[lbt.b])
            op("dve", lambda e: e.tensor_tensor(lbt.t[:, 1, :], vec.t[:, 80:88], vec.t[:, 72:80], ALU.subtract),
               reads=[vec.b], writes=[lbt.b])
            op("act", lambda e: e.activation(lbt.t[:, 1, :], lbt.t[:, 1, :], AF.Sigmoid), reads=[lbt.b], writes=[lbt.b])
            op("dve", lambda e: e.tensor_scalar(omlt.t[:], lbt.t[:], -1.0, 1.0, ALU.mult, ALU.add), reads=[lbt.b], writes=[omlt.b])
            op("dve", lambda e: e.tensor_scalar(nomlt.t[:], lbt.t[:], 1.0, -1.0, ALU.mult, ALU.add), reads=[lbt.b], writes=[nomlt.b])
            c.release([stg.b])

        def norm(st, gcol0):
            ts = slice(st * ST, (st + 1) * ST)
            for dc in range(8):
                s_ = sq[dc % 2]
                op("act", lambda e: e.activation(s_.t[:], hT[:, dc, ts], AF.Square), reads=[bh[st]], writes=[s_.b])
                op("pe", lambda e: e.matmul(P[:, 3, :], onesb.t[:], s_.t[:], start=(dc == 0), stop=(dc == 7)),
                   reads=[onesb.b, s_.b], writes=[pb[3]], pe_accum=True)
            op("act", lambda e: e.activation(rstd.t[:], P[:, 3, :], AF.Sqrt, bias=EPS, scale=1.0 / D), reads=[pb[3]], writes=[rstd.b])
            op("dve", lambda e: e.reciprocal(rstd.t[:], rstd.t[:]), reads=[rstd.b], writes=[rstd.b])
            for dc in range(8):
                op("dve", lambda e: e.scalar_tensor_tensor(hn.t[:, dc, :], hT[:, dc, ts], vec.t[:, gcol0 + dc:gcol0 + dc + 1],
                                                           rstd.t[:], ALU.mult, ALU.mult),
                   reads=[bh[st], vec.b, rstd.b], writes=[hn.b])

        def mm_fm(bank, wt, wb_, col0, rhs_tl, rhs_ap_fn, nk=8):
            for k in range(nk):
                op("pe", lambda e: e.matmul(P[:, bank, :], wt[:, k, col0:col0 + 128], rhs_ap_fn(k), start=(k == 0), stop=(k == nk - 1)),
                   reads=list(wb_) + [rhs_tl.b], writes=[pb[bank]], pe_accum=True)

        def out_proj(w_dram, src_tl, st, banks=(0, 1)):
            ts = slice(st * ST, (st + 1) * ST)
            it = 0
            tiles = []
            for half in range(2):
                wt, wsb = wnext()
                wload(wt[:], w_dram[:, half * 512:(half + 1) * 512].rearrange("(k p) n -> p k n", p=128), wsb)
                tiles.append((wt, wsb))
            for half in range(2):
                wt, wsb = tiles[half]
                for dq in range(4):
                    dc = 4 * half + dq
                    bank = banks[it % 2]; it += 1
                    mm_fm(bank, wt, wsb, dq * 128, src_tl, lambda k: src_tl.t[:, k, :])
                    op("dve", lambda e: e.tensor_tensor(hT[:, dc, ts], P[:, bank, :], hT[:, dc, ts], ALU.add),
                       reads=[pb[bank], bh[st]], writes=[bh[st]])

        def ffn_phase(layer):
            with ExitStack() as ps_:
                g = sb(ps_, "g", [128, 22, ST], BF16)
                ag = [sb(ps_, f"ag{i}", [128, ST]) for i in range(2)]
                au = [sb(ps_, f"au{i}", [128, ST]) for i in range(2)]
                carry = [sb(ps_, f"carry{i}", [128, 44, 2]) for i in range(2)]
                bnd = sb(ps_, "bnd", [128, 44, 2]); tmpb = sb(ps_, "tmpb", [128, 44])
                wup = ffn_w_up[layer]
                wdn = ffn_w_down[layer]
                cw = lambda k, ci: convw.t[:, (layer * 3 + k) * 44 + ci:(layer * 3 + k) * 44 + ci + 1]
                cwv = lambda k: convw.t[:, (layer * 3 + k) * 44:(layer * 3 + k) * 44 + 44]

                def load_up(jj):
                    wt, wsb = wnext()
                    wload(wt[:, :, 0:256], wup[:, 256 * jj:256 * jj + 256].rearrange("(k p) n -> p k n", p=128), [wsb[0]])
                    wload(wt[:, :, 256:512], wup[:, DFF + 256 * jj:DFF + 256 * jj + 256].rearrange("(k p) n -> p k n", p=128), [wsb[1]])
                    return wt, wsb

                def load_dn(half, kg):
                    nk = min(8, 22 - 8 * kg)
                    wt, wsb = wnext()
                    wload(wt[:, 0:nk, :], wdn[kg * 1024:kg * 1024 + nk * 128, half * 512:(half + 1) * 512].rearrange("(k p) n -> p k n", p=128), wsb)
                    return wt, wsb, nk

                for st in range(NST):
                    ts = slice(st * ST, (st + 1) * ST)
                    norm(st, 32 + layer * 8)
                    cprev = carry[(st + 1) % 2]; cnew = carry[st % 2]
                    if st > 0:
                        op("pool", lambda e: e.tensor_tensor(tmpb.t[:], cprev.t[:, :, 1], cwv(1), ALU.mult), reads=[cprev.b, convw.b], writes=[tmpb.b])
                        op("pool", lambda e: e.tensor_tensor(bnd.t[:, :, 0], cprev.t[:, :, 0], cwv(0), ALU.mult), reads=[cprev.b, convw.b], writes=[bnd.b])
                        op("pool", lambda e: e.tensor_tensor(bnd.t[:, :, 0], bnd.t[:, :, 0], tmpb.t[:], ALU.add), reads=[bnd.b, tmpb.b], writes=[bnd.b])
                        op("pool", lambda e: e.tensor_tensor(bnd.t[:, :, 1], cprev.t[:, :, 1], cwv(0), ALU.mult), reads=[cprev.b, convw.b], writes=[bnd.b])

                    def conv(bank, ci, a):
                        op("act", lambda e: e.activation(a.t[:], P[:, bank, :], AF.Identity,
                                                         bias=convb.t[:, layer * 44 + ci:layer * 44 + ci + 1], scale=cw(2, ci)),
                           reads=[pb[bank], convw.b, convb.b], writes=[a.b])
                        if st > 0:
                            op("dve", lambda e: e.tensor_tensor(a.t[:, 0:2], a.t[:, 0:2], bnd.t[:, ci, :], ALU.add), reads=[a.b, bnd.b], writes=[a.b])
                        op("dve", lambda e: e.scalar_tensor_tensor(a.t[:, 1:ST], P[:, bank, 0:ST - 1], cw(1, ci), a.t[:, 1:ST], ALU.mult, ALU.add),
                           reads=[pb[bank], convw.b, a.b], writes=[a.b])
                        op("dve", lambda e: e.scalar_tensor_tensor(a.t[:, 2:ST], P[:, bank, 0:ST - 2], cw(0, ci), a.t[:, 2:ST], ALU.mult, ALU.add),
                           reads=[pb[bank], convw.b, a.b], writes=[a.b])
                        op("act", lambda e: e.copy(cnew.t[:, ci, :], P[:, bank, ST - 2:ST]), reads=[pb[bank]], writes=[cnew.b])

                    pend = [load_up(0), load_up(1)]
                    for jj in range(11):
                        wt, wsb = pend.pop(0)
                        if jj + 2 < 11:
                            pend.append(load_up(jj + 2))
                        for sub in range(2):
                            j = 2 * jj + sub
                            bg_, bu_ = 2 * (j % 2), 2 * (j % 2) + 1
                            mm_fm(bg_, wt, [wsb[0]], sub * 128, hn, lambda k: hn.t[:, k, :])
                            mm_fm(bu_, wt, [wsb[1]], 256 + sub * 128, hn, lambda k: hn.t[:, k, :])
                            a_g = ag[j % 2]; a_u = au[j % 2]
                            conv(bg_, j, a_g)
                            conv(bu_, 22 + j, a_u)
                            op("act", lambda e: e.activation(a_g.t[:], a_g.t[:], AF.Silu), reads=[a_g.b], writes=[a_g.b])
                            op("dve", lambda e: e.tensor_tensor(g.t[:, j, :], a_g.t[:], a_u.t[:], ALU.mult), reads=[a_g.b, a_u.b], writes=[g.b])
                    seqd = [(h_, k_) for h_ in range(2) for k_ in range(3)]
                    pend = [load_dn(*seqd[0]), load_dn(*seqd[1])]
                    for idx, (half, kg) in enumerate(seqd):
                        wt, wsb, nk = pend.pop(0)
                        if idx + 2 < len(seqd):
                            pend.append(load_dn(*seqd[idx + 2]))
                        for dq in range(4):
                            for kk in range(nk):
                                op("pe", lambda e: e.matmul(P[:, 4 + dq, :], wt[:, kk, dq * 128:(dq + 1) * 128], g.t[:, 8 * kg + kk, :],
                                                            start=(kg == 0 and kk == 0), stop=(kg == 2 and kk == nk - 1)),
                                   reads=list(wsb) + [g.b], writes=[pb[4 + dq]], pe_accum=True)
                        if kg == 2:
                            for dq in range(4):
                                dc = 4 * half + dq
                                op("dve", lambda e: e.tensor_tensor(hT[:, dc, ts], P[:, 4 + dq, :], hT[:, dc, ts], ALU.add),
                                   reads=[pb[4 + dq], bh[st]], writes=[bh[st]])
                c.release([g.b, bnd.b, tmpb.b] + [t.b for t in ag + au + carry])

        def hgrn_phase(layer):
            j = layer // 2
            win = hgrn_w_in[j]
            with ExitStack() as ps_:
                qtil = sb(ps_, "qtil", [128, 8, ST], BF16); ktil = sb(ps_, "ktil", [128, 8, ST], BF16)
                khat = sb(ps_, "khat", [128, 4, D], BF16); itok = sb(ps_, "itok", [128, 4, D], BF16)
                sgg = sb(ps_, "sgg", [128, 4, D], BF16)
                E2 = [sb(ps_, f"E{i}", [128, ST]) for i in range(2)]
                elast = sb(ps_, "elast", [128, 8, 8])
                tsig = sb(ps_, "tsig", [128, ST]); tlf = sb(ps_, "tlf", [128, ST]); tk = sb(ps_, "tk", [128, ST])
                tb = sb(ps_, "tb", [128, ST]); tei = sb(ps_, "tei", [128, ST]); tnb = sb(ps_, "tnb", [128, ST])
                tsg = sb(ps_, "tsg", [128, ST])
                S = [sb(ps_, f"S{i}", [128, 8, 128]) for i in range(2)]
                Sbf = [sb(ps_, f"Sbf{i}", [128, 8, 128], BF16) for i in range(2)]
                ATm = [sb(ps_, f"ATm{i}", [128, 8, 128], BF16) for i in range(2)]
                ss = sb(ps_, "ss", [128, 8]); yg = sb(ps_, "yg", [128, D])
                oT = hn
                allb = [t.b for t in [qtil, ktil, khat, itok, sgg, elast, tsig, tlf, tk, tb, tei, tnb, tsg, ss, yg] + E2 + S + Sbf + ATm]
                for s_ in S:
                    op("dve", lambda e: e.memset(s_.t[:], 0.0), writes=[s_.b])
                for s_ in Sbf:
                    op("dve", lambda e: e.memset(s_.t[:], 0.0), writes=[s_.b])
                pbank = {"i": 0}

                def nb_():
                    pbank["i"] ^= 1
                    return pbank["i"]

                def lw(col0):
                    wt, wsb = wnext()
                    wload(wt[:], win[:, col0:col0 + 512].rearrange("(k p) n -> p k n", p=128), wsb)
                    return wt, wsb

                cg = 0
                for st in range(NST):
                    ts = slice(st * ST, (st + 1) * ST)
                    norm(st, layer * 8)
                    order = [1024, 0, 1536, 512, 2048, 2560, 3072, 3584]
                    pend = [lw(order[0])]
                    nxt = [1]

                    def take():
                        r = pend.pop(0)
                        if nxt[0] < len(order):
                            pend.append(lw(order[nxt[0]]))
                            nxt[0] += 1
                        return r

                    for grp in range(2):
                        wf, wfb = take()
                        wq, wqb = take()
                        for hh in range(4):
                            h = 4 * grp + hh
                            Eh = E2[h % 2]
                            bk = nb_()
                            mm_fm(bk, wf, wfb, hh * 128, hn, lambda k: hn.t[:, k, :])
                            op("act", lambda e: e.activation(tsig.t[:], P[:, bk, :], AF.Sigmoid), reads=[pb[bk]], writes=[tsig.b])
                            op("act", lambda e: e.activation(tlf.t[:], tsig.t[:], AF.Ln, bias=lbt.t[:, j, h:h + 1], scale=omlt.t[:, j, h:h + 1]),
                               reads=[tsig.b, lbt.b, omlt.b], writes=[tlf.b])
                            op("dve", lambda e: e.tensor_scalar(tk.t[:], tsig.t[:], nomlt.t[:, j, h:h + 1], omlt.t[:, j, h:h + 1], ALU.mult, ALU.add),
                               reads=[tsig.b, nomlt.b, omlt.b], writes=[tk.b])
                            op("dve", lambda e: e.tensor_tensor_scan(tb.t[:], scanmask.t[:], tlf.t[:], 0.0, ALU.mult, ALU.add),
                               reads=[scanmask.b, tlf.b], writes=[tb.b])
                            op("act", lambda e: e.activation(Eh.t[:], tb.t[:], AF.Exp), reads=[tb.b], writes=[Eh.b])
                            op("pool", lambda e: e.tensor_scalar(tei.t[:], tb.t[:], 1.0e30, -80.0, ALU.min, ALU.max), reads=[tb.b], writes=[tei.b])
                            op("act", lambda e: e.activation(tei.t[:], tei.t[:], AF.Exp, scale=-1.0), reads=[tei.b], writes=[tei.b])
                            b3 = tb.t[:].rearrange("p (n c) -> p n c", c=64)
                            op("pool", lambda e: e.tensor_tensor(tnb.t[:].rearrange("p (n c) -> p n c", c=64),
                                                                 b3[:, :, 63:64].to_broadcast([128, 8, 64]), b3, ALU.subtract),
                               reads=[tb.b], writes=[tnb.b])
                            op("act", lambda e: e.activation(tnb.t[:], tnb.t[:], AF.Exp), reads=[tnb.b], writes=[tnb.b])
                            op("pool", lambda e: e.tensor_copy(elast.t[:, h, :], Eh.t[:].rearrange("p (n c) -> p n c", c=64)[:, :, 63]),
                               reads=[Eh.b], writes=[elast.b])
                            op("pool", lambda e: e.tensor_tensor(ktil.t[:, h, :], tk.t[:], tei.t[:], ALU.mult), reads=[tk.b, tei.b], writes=[ktil.b])
                            op("pool", lambda e: e.tensor_tensor(tnb.t[:], tk.t[:], tnb.t[:], ALU.mult), reads=[tk.b, tnb.b], writes=[tnb.b])
                            for n in range(4):
                                op("pe", lambda e: e.transpose(P[:, 2, n * 128:(n + 1) * 128], tnb.t[:, n * 128:(n + 1) * 128], ident.t[:]),
                                   reads=[tnb.b, ident.b], writes=[pb[2]], pe_accum=True)
                            op("act", lambda e: e.copy(khat.t[:, :, h * 128:(h + 1) * 128], P[:, 2, :].rearrange("p (n d) -> p n d", d=128)),
                               reads=[pb[2]], writes=[khat.b])
                            bk = nb_()
                            mm_fm(bk, wq, wqb, hh * 128, hn, lambda k: hn.t[:, k, :])
                            op("dve", lambda e: e.tensor_tensor(qtil.t[:, h, :], P[:, bk, :], Eh.t[:], ALU.mult), reads=[pb[bk], Eh.b], writes=[qtil.b])
                    for kind in range(2):
                        for grp in range(2):
                            wt, wtb = take()
                            for n in range(4):
                                bk = nb_()
                                for k in range(8):
                                    op("pe", lambda e: e.matmul(P[:, bk, :], hn.t[:, k, n * 128:(n + 1) * 128], wt[:, k, :], start=(k == 0), stop=(k == 7)),
                                       reads=[hn.b] + list(wtb), writes=[pb[bk]], pe_accum=True)
                                if kind == 0:
                                    op("act", lambda e: e.copy(itok.t[:, n, grp * 512:(grp + 1) * 512], P[:, bk, :]), reads=[pb[bk]], writes=[itok.b])
                                else:
                                    op("act", lambda e: e.activation(tsg.t[:], P[:, bk, :], AF.Silu), reads=[pb[bk]], writes=[tsg.b])
                                    op("pool", lambda e: e.tensor_tensor(sgg.t[:, n, grp * 512:(grp + 1) * 512].rearrange("p (h v) -> p h v", v=128),
                                                                         tsg.t[:].rearrange("p (h v) -> p h v", v=128),
                                                                         gnb.t[:, j, :].unsqueeze(1).to_broadcast([128, 4, 128]), ALU.mult),
                                       reads=[tsg.b, gnb.b], writes=[sgg.b])
                    for n in range(4):
                        am = ATm[n % 2]
                        for h in range(8):
                            op("pe", lambda e: e.matmul(P[:, 2 + h // 4, (h % 4) * 128:(h % 4 + 1) * 128], ktil.t[:, h, n * 128:(n + 1) * 128],
                                                        qtil.t[:, h, n * 128:(n + 1) * 128], start=(h % 4 == 0), stop=True, skip_group_check=True),
                               reads=[ktil.b, qtil.b], writes=[pb[2 + h // 4]], pe_accum=True)
                        op("dve", lambda e: e.tensor_tensor(am.t[:], P[:, 2:4, :].rearrange("p b (h t) -> p (b h) t", t=128),
                                                            maskbd.t[:].unsqueeze(1).to_broadcast([128, 8, 128]), ALU.mult),
                           reads=[pb[2], pb[3], maskbd.b], writes=[am.b])
                        for jc in range(2):
                            rows = slice(64 * jc, 64 * jc + 64)
                            col0 = n * 128 + 64 * jc
                            vb = (6, 7) if cg % 2 == 0 else (0, 1)
                            for h in range(8):
                                op("pe", lambda e: e.matmul(P[:, vb[h // 4], (h % 4) * 128:(h % 4 + 1) * 128], khat.t[rows, n, h * 128:(h + 1) * 128],
                                                            itok.t[rows, n, h * 128:(h + 1) * 128], start=(h % 4 == 0), stop=True, skip_group_check=True),
                                   reads=[khat.b, itok.b], writes=[pb[vb[h // 4]]], pe_accum=True)
                            sprev = Sbf[(cg + 1) % 2]
                            for h in range(8):
                                ob = 4 + h // 4
                                oc = slice((h % 4) * 128, (h % 4 + 1) * 128)
                                op("pe", lambda e: e.matmul(P[rows, ob, oc], am.t[rows, h, 64 * jc:64 * jc + 64], itok.t[rows, n, h * 128:(h + 1) * 128],
                                                            start=True, stop=(cg == 0), skip_group_check=True),
                                   reads=[am.b, itok.b], writes=[pb[ob]], pe_accum=True)
                                if cg > 0:
                                    op("pe", lambda e: e.matmul(P[rows, ob, oc], qtil.t[:, h, col0:col0 + 64], sprev.t[:, h, :],
                                                                start=False, stop=True, skip_group_check=True),
                                       reads=[qtil.b, sprev.b], writes=[pb[ob]], pe_accum=True)
                            scur = S[cg % 2]; sold = S[(cg + 1) % 2]
                            for h in range(8):
                                op("dve", lambda e: e.scalar_tensor_tensor(scur.t[:, h, :], sold.t[:, h, :], elast.t[:, h, 2 * n + jc:2 * n + jc + 1],
                                                                           P[:, vb[h // 4], (h % 4) * 128:(h % 4 + 1) * 128], ALU.mult, ALU.add),
                                   reads=[sold.b, elast.b, pb[vb[h // 4]]], writes=[scur.b])
                            op("act", lambda e: e.copy(Sbf[cg % 2].t[:], scur.t[:]), reads=[scur.b], writes=[Sbf[cg % 2].b])
                            cg += 1
                        o3 = P[:, 4:6, :].rearrange("p b (h v) -> p (b h) v", v=128)
                        op("act", lambda e: e.activation(yg.t[:].rearrange("p (h v) -> p h v", v=128), o3, AF.Square), reads=[pb[4], pb[5]], writes=[yg.b])
                        op("dve", lambda e: e.tensor_reduce(ss.t[:], yg.t[:].rearrange("p (h v) -> p h v", v=128), AX.X, ALU.add), reads=[yg.b], writes=[ss.b])
                        op("act", lambda e: e.activation(ss.t[:], ss.t[:], AF.Sqrt, bias=EPS, scale=1.0 / 128), reads=[ss.b], writes=[ss.b])
                        op("dve", lambda e: e.reciprocal(ss.t[:], ss.t[:]), reads=[ss.b], writes=[ss.b])
                        for h in range(8):
                            op("dve", lambda e: e.scalar_tensor_tensor(yg.t[:, h * 128:(h + 1) * 128], P[:, 4 + h // 4, (h % 4) * 128:(h % 4 + 1) * 128],
                                                                       ss.t[:, h:h + 1], sgg.t[:, n, h * 128:(h + 1) * 128], ALU.mult, ALU.mult),
                               reads=[pb[4 + h // 4], ss.b, sgg.b], writes=[yg.b])
                        for dc in range(8):
                            op("pe", lambda e: e.transpose(P[:, 2 + dc // 4, (dc % 4) * 128:(dc % 4 + 1) * 128], yg.t[:, dc * 128:(dc + 1) * 128], ident.t[:]),
                               reads=[yg.b, ident.b], writes=[pb[2 + dc // 4]], pe_accum=True)
                        op("act", lambda e: e.copy(oT.t[:, :, n * 128:(n + 1) * 128], P[:, 2:4, :].rearrange("p b (h t) -> p (b h) t", t=128)),
                           reads=[pb[2], pb[3]], writes=[oT.b])
                    out_proj(hgrn_w_out[j], oT, st)
                c.release(allb)

        def dsa_phase(layer):
            jd = layer // 2
            win = dsa_w_in[jd]
            wv = lambda c0, w_: win[:, c0:c0 + w_].rearrange("(k p) n -> p k n", p=128)
            with ExitStack() as ps_:
                Yp = sb(ps_, "Yp", [128, 16, 2, 128], BF16)
                kdup = sb(ps_, "kdup", [128, 2, L], BF16); kidx = sb(ps_, "kidx", [128, L], BF16)
                vaug = sb(ps_, "vaug", [128, 16, 2, 65], BF16)
                wsm = sb(ps_, "wsm", [128, 8, 8])
                qT = sb(ps_, "qT", [128, 8, ST], BF16)
                wpos = sb(ps_, "wpos", [128, ST]); wneg = sb(ps_, "wneg", [128, ST])
                qpos = sb(ps_, "qpos", [128, 4, ST], BF16); qneg = sb(ps_, "qneg", [128, 4, ST], BF16)
                scores = sb(ps_, "scores", [128, L]); work = sb(ps_, "work", [128, L]); m8 = sb(ps_, "m8", [128, 8])
                M01 = [sb(ps_, f"M01_{i}", [128, 16, 128], BF16) for i in range(2)]
                MBn = [sb(ps_, f"MBn_{i}", [128, 2, 128]) for i in range(2)]
                ym = sb(ps_, "ym", [128, 8, 128])
                lg = sb(ps_, "lg", [128, 8, 128])
                itc = [0]
                pex = [sb(ps_, f"pex{i}", [128, 8, 128], BF16) for i in range(2)]
                rden = sb(ps_, "rden", [128, 16]); atok = sb(ps_, "atok", [128, D])
                aT = hn
                allb = [t.b for t in [Yp, kdup, kidx, vaug, wsm, qT, wpos, wneg, qpos, qneg, scores, work, m8, ym, lg, rden, atok] + M01 + MBn + pex]
                for g in range(8):
                    blk = g // 4
                    for q in range(32):
                        pt = 32 * (g % 4) + q
                        u0 = 255 - pt - 128 * blk
                        op("pe", lambda e: e.matmul(P[:, 1, q * 16:(q + 1) * 16], gr.t[:, u0:u0 + 128], rb.t[:, :],
                                                    start=(q == 0), stop=True, skip_group_check=True),
                           reads=[gr.b, rb.b], writes=[pb[1]], pe_accum=True)
                    pt0 = 32 * (g % 4)
                    op("dve", lambda e: e.tensor_tensor(Yp.t[:, :, blk, pt0:pt0 + 32],
                                                        P[:, 1, :].rearrange("p (t h) -> p h t", h=16),
                                                        cb.t[:].unsqueeze(2).to_broadcast([128, 16, 32]), ALU.subtract),
                       reads=[pb[1], cb.b], writes=[Yp.b])
                with nc.allow_non_contiguous_dma(reason="tiny w_idx column block"):
                    c.dma("sp", wsm.t[:], wv(1856, 8), writes=[wsm.b])
                op("dve", lambda e: e.memset(vaug.t[:], 1.0), writes=[vaug.b])
                pbank = {"i": 0}

                def nb_():
                    pbank["i"] = (pbank["i"] + 1) % 4
                    return pbank["i"]

                for st in range(NST):
                    ts = slice(st * ST, (st + 1) * ST)
                    norm(st, layer * 8)
                    wkv, wkvb = wnext()
                    for i_, (sc, dc_) in enumerate([(1024, 0), (1024, 64), (1088, 128), (1088, 192), (1792, 256), (1792, 320)]):
                        wload(wkv[:, :, dc_:dc_ + 64], wv(sc, 64), [wkvb[i_]])
                    wload(wkv[:, :, 384:512], wv(1152, 128), [wkvb[6], wkvb[7]])
                    for cc in range(3):
                        bk = nb_()
                        mm_fm(bk, wkv, wkvb, cc * 128, hn, lambda k: hn.t[:, k, :])
                        if cc < 2:
                            op("act", lambda e: e.copy(kdup.t[:, cc, ts], P[:, bk, :]), reads=[pb[bk]], writes=[kdup.b])
                        else:
                            op("act", lambda e: e.copy(kidx.t[:, ts], P[:, bk, :]), reads=[pb[bk]], writes=[kidx.b])
                    for n in range(4):
                        bk = nb_()
                        for k in range(8):
                            op("pe", lambda e: e.matmul(P[:, bk, 0:128], hn.t[:, k, n * 128:(n + 1) * 128], wkv[:, k, 384:512], start=(k == 0), stop=(k == 7)),
                               reads=[hn.b] + list(wkvb), writes=[pb[bk]], pe_accum=True)
                        op("act", lambda e: e.copy(vaug.t[:, 4 * st + n, :, 0:64], P[:, bk, 0:128].rearrange("p (a d) -> p a d", d=64)),
                           reads=[pb[bk]], writes=[vaug.b])
                    wtq = []
                    for col0 in (0, 512):
                        wt, wsb = wnext()
                        wload(wt[:], wv(col0, 512), wsb)
                        wtq.append((wt, wsb))
                    for half in range(2):
                        wt, wsb = wtq[half]
                        for cq in range(4):
                            bk = nb_()
                            mm_fm(bk, wt, wsb, cq * 128, hn, lambda k: hn.t[:, k, :])
                            op("act", lambda e: e.activation(qT.t[:, 4 * half + cq, :], P[:, bk, :], AF.Copy, scale=0.125), reads=[pb[bk]], writes=[qT.b])
                    ww, wwb = wnext()
                    op("dve", lambda e: e.tensor_scalar(ww[:].rearrange("p k (h r) -> p k h r", r=64),
                                                        wsm.t[:].unsqueeze(3).to_broadcast([128, 8, 8, 64]),
                                                        1.0 / math.sqrt(512.0), None, ALU.mult),
                       reads=[wsm.b], writes=wwb)
                    wqi, wqib = wnext()
                    wload(wqi[:], wv(1280, 512), wqib)
                    for cw_ in range(4):
                        bk = nb_()
                        mm_fm(bk, ww, wwb, cw_ * 128, hn, lambda k: hn.t[:, k, :])
                        op("dve", lambda e: e.tensor_scalar(wpos.t[:], P[:, bk, :], 0.0, None, ALU.max), reads=[pb[bk]], writes=[wpos.b])
                        op("dve", lambda e: e.tensor_scalar(wneg.t[:], P[:, bk, :], 0.0, None, ALU.min), reads=[pb[bk]], writes=[wneg.b])
                        bk = nb_()
                        mm_fm(bk, wqi, wqib, cw_ * 128, hn, lambda k: hn.t[:, k, :])
                        op("dve", lambda e: e.tensor_tensor(qpos.t[:, cw_, :], P[:, bk, :], wpos.t[:], ALU.mult), reads=[pb[bk], wpos.b], writes=[qpos.b])
                        op("dve", lambda e: e.tensor_tensor(qneg.t[:, cw_, :], P[:, bk, :], wneg.t[:], ALU.mult), reads=[pb[bk], wneg.b], writes=[qneg.b])
                    def stage_A(n):
                        Tg = 4 * st + n
                        nk = 128 * (Tg + 1)
                        qc = slice(n * 128, (n + 1) * 128)
                        for kbk in range((nk + 511) // 512):
                            kw = min(512, nk - 512 * kbk)
                            ks = slice(512 * kbk, 512 * kbk + kw)
                            first = True
                            for ih in range(8):
                                r0 = (ih % 2) * 64
                                for sgn in range(2):
                                    qq = qpos if sgn == 0 else qneg
                                    bk = (ih % 2) * 2 + sgn
                                    op("pe", lambda e: e.matmul(P[:, bk, 0:kw], qq.t[r0:r0 + 64, ih // 2, qc], kidx.t[r0:r0 + 64, ks], start=True, stop=True),
                                       reads=[qq.b, kidx.b], writes=[pb[bk]], pe_accum=True)
                                    aop = ALU.max if sgn == 0 else ALU.min
                                    if first:
                                        op("dve", lambda e: e.tensor_scalar(scores.t[:, ks], P[:, bk, 0:kw], 0.0, None, aop), reads=[pb[bk]], writes=[scores.b])
                                        first = False
                                    else:
                                        op("dve", lambda e: e.scalar_tensor_tensor(scores.t[:, ks], P[:, bk, 0:kw], 0.0, scores.t[:, ks], aop, ALU.add),
                                           reads=[pb[bk], scores.b], writes=[scores.b])
                        op("pool", lambda e: e.tensor_tensor(scores.t[:, 128 * Tg:128 * Tg + 128], scores.t[:, 128 * Tg:128 * Tg + 128],
                                                             causal.t[:], ALU.add),
                           reads=[scores.b, causal.b], writes=[scores.b])

                    def stage_B(n):
                        Tg = 4 * st + n
                        nk = 128 * (Tg + 1)
                        if Tg >= 2:
                            for r_ in range(32):
                                src = scores if r_ == 0 else work
                                op("dve", lambda e: e.max(m8.t[:], src.t[:, 0:nk]), reads=[src.b], writes=[m8.b])
                                if r_ < 31:
                                    op("dve", lambda e: e.match_replace(work.t[:, 0:nk], m8.t[:], src.t[:, 0:nk], NEG), reads=[m8.b, src.b], writes=[work.b])
                            op("dve", lambda e: e.tensor_scalar(work.t[:, 0:nk], scores.t[:, 0:nk], m8.t[:, 7:8], None, ALU.is_ge),
                               reads=[scores.b, m8.b], writes=[work.b])
                        else:
                            op("dve", lambda e: e.tensor_scalar(work.t[:, 0:nk], scores.t[:, 0:nk], -1.0e29, None, ALU.is_ge), reads=[scores.b], writes=[work.b])

                    def stage_C(n):
                        Tg = 4 * st + n
                        m01 = M01[Tg % 2]; mbn = MBn[Tg % 2]
                        kb = 0
                        while kb <= Tg:
                            cnt = min(4, Tg + 1 - kb)
                            for q_ in range(cnt):
                                op("pe", lambda e: e.transpose(P[:, 4, q_ * 128:(q_ + 1) * 128], work.t[:, (kb + q_) * 128:(kb + q_ + 1) * 128], ident.t[:]),
                                   reads=[work.b, ident.b], writes=[pb[4]], pe_accum=True)
                            op("dve", lambda e: e.tensor_copy(m01.t[:, kb:kb + cnt, :], P[:, 4, 0:cnt * 128].rearrange("p (a t) -> p a t", t=128)),
                               reads=[pb[4]], writes=[m01.b])
                            for q_ in range(cnt):
                                blk = Tg - (kb + q_)
                                if blk <= 1:
                                    op("dve", lambda e: e.tensor_scalar(mbn.t[:, blk, :], P[:, 4, q_ * 128:(q_ + 1) * 128], BIG, -BIG, ALU.mult, ALU.add),
                                       reads=[pb[4]], writes=[mbn.b])
                            kb += cnt

                    def stage_D(n):
                        Tg = 4 * st + n
                        qc = slice(n * 128, (n + 1) * 128)
                        m01 = M01[Tg % 2]; mbn = MBn[Tg % 2]
                        kbs = [Tg] + ([Tg - 1] if Tg >= 1 else []) + list(range(0, Tg - 1))
                        for kb in kbs:
                            near = kb >= Tg - 1
                            blk = Tg - kb
                            for hg in range(2):
                                it = itc[0]; itc[0] += 1
                                banks = (0, 1) if it % 2 == 0 else (2, 3)
                                r_ = it % 2
                                for hh in range(8):
                                    h = 8 * hg + hh
                                    r0 = (h % 2) * 64
                                    op("pe", lambda e: e.matmul(P[:, banks[hh % 2], (hh // 2) * 128:(hh // 2 + 1) * 128], kdup.t[r0:r0 + 64, h // 8, kb * 128:(kb + 1) * 128],
                                                                qT.t[r0:r0 + 64, h // 2, qc], start=(hh // 2 == 0), stop=True, skip_group_check=True),
                                       reads=[kdup.b, qT.b], writes=[pb[banks[hh % 2]]], pe_accum=True)
                                lsrc = P[:, banks[0]:banks[0] + 2, :].rearrange("p b (h t) -> p (b h) t", t=128)
                                if near:
                                    op("pool", lambda e: e.tensor_tensor(ym.t[:].rearrange("p (two a) t -> p two a t", two=2),
                                                                         Yp.t[:, 8 * hg:8 * hg + 8, blk, :].rearrange("p (a two) t -> p two a t", two=2),
                                                                         mbn.t[:, blk, :].unsqueeze(1).unsqueeze(1).to_broadcast([128, 2, 4, 128]), ALU.add),
                                       reads=[Yp.b, mbn.b], writes=[ym.b])
                                    op("dve", lambda e: e.tensor_tensor(lg.t[:], lsrc, ym.t[:], ALU.add),
                                       reads=[pb[banks[0]], pb[banks[1]], ym.b], writes=[lg.b])
                                    op("act", lambda e: e.activation(pex[r_].t[:], lg.t[:], AF.Exp), reads=[lg.b], writes=[pex[r_].b])
                                else:
                                    op("act", lambda e: e.activation(pex[r_].t[:], lsrc, AF.Exp), reads=[pb[banks[0]], pb[banks[1]]], writes=[pex[r_].b])
                                    if KDBG != "nopoolmask":
                                        op("pool", lambda e: e.tensor_tensor(pex[r_].t[:], pex[r_].t[:], m01.t[:, kb, :].unsqueeze(1).to_broadcast([128, 8, 128]), ALU.mult),
                                           reads=[pex[r_].b, m01.b], writes=[pex[r_].b])
                                for hh in range(8):
                                    h = 8 * hg + hh
                                    ob = 5 + h // 6
                                    oc = (h % 6) * 65
                                    op("pe", lambda e: e.matmul(P[:, ob, oc:oc + 65], pex[r_].t[:, (hh % 2) * 4 + hh // 2, :], vaug.t[:, kb, h // 8, :],
                                                                start=(kb == kbs[0] and h % 6 == 0), stop=(kb == kbs[-1]), skip_group_check=True),
                                       reads=[pex[r_].b, vaug.b], writes=[pb[ob]], pe_accum=True)

                    def stage_E(n):
                        qc = slice(n * 128, (n + 1) * 128)
                        for b3 in range(3):
                            nh = 6 if b3 < 2 else 4
                            o3 = P[:, 5 + b3, 0:nh * 65].rearrange("p (h d) -> p h d", d=65)
                            op("dve", lambda e: e.reciprocal(rden.t[:, 6 * b3:6 * b3 + nh], o3[:, :, 64]), reads=[pb[5 + b3]], writes=[rden.b])
                            op("dve", lambda e: e.tensor_tensor(atok.t[:, 384 * b3:384 * b3 + nh * 64].rearrange("p (h d) -> p h d", d=64), o3[:, :, 0:64],
                                                                rden.t[:, 6 * b3:6 * b3 + nh].unsqueeze(2).to_broadcast([128, nh, 64]), ALU.mult),
                               reads=[pb[5 + b3], rden.b], writes=[atok.b])
                        for dc in range(8):
                            op("pe", lambda e: e.transpose(P[:, dc // 4, (dc % 4) * 128:(dc % 4 + 1) * 128], atok.t[:, dc * 128:(dc + 1) * 128], ident.t[:]),
                               reads=[atok.b, ident.b], writes=[pb[dc // 4]], pe_accum=True)
                        op("act", lambda e: e.copy(aT.t[:, :, qc], P[:, 0:2, :].rearrange("p b (h t) -> p (b h) t", t=128)), reads=[pb[0], pb[1]], writes=[aT.b])

                    if KDBG.startswith("seq"):
                        for n in range(4):
                            stage_A(n); stage_B(n)
                            if KDBG == "seqB":
                                continue
                            stage_C(n)
                            if KDBG == "seqC":
                                continue
                            stage_D(n); stage_E(n)
                    elif dbg_stop > 0:
                        stage_A(0); stage_B(0); stage_C(0)
                        for n in range(4):
                            if n + 1 < 4:
                                stage_A(n + 1)
                            stage_D(n)
                            if n + 1 < 4:
                                stage_B(n + 1)
                            stage_E(n)
                            if n + 1 < 4:
                                stage_C(n + 1)
                    out_proj(dsa_w_out[jd], aT, st, banks=(2, 3))
                c.release(allb)

        for s in range(nseq):
            with ExitStack() as ps_:
                xs = [sb(ps_, f"xs{i}", [128, 4, D]) for i in range(2)]
                it = 0
                for st in range(NST):
                    xt = xs[st % 2]
                    c.dma("sp", xt.t[:], x[s, st * ST:(st + 1) * ST, :].rearrange("(n p) d -> p n d", p=128), writes=[xt.b])
                    for dc in range(8):
                        bk = it % 2; it += 1
                        for n in range(4):
                            op("pe", lambda e: e.transpose(P[:, bk, n * 128:(n + 1) * 128], xt.t[:, n, dc * 128:(dc + 1) * 128], ident.t[:]),
                               reads=[xt.b, ident.b], writes=[pb[bk]], pe_accum=True)
                        if dc % 2 == 0:
                            op("act", lambda e: e.copy(hT[:, dc, st * ST:(st + 1) * ST], P[:, bk, :]), reads=[pb[bk]], writes=[bh[st]])
                        else:
                            op("dve", lambda e: e.tensor_copy(hT[:, dc, st * ST:(st + 1) * ST], P[:, bk, :]), reads=[pb[bk]], writes=[bh[st]])
                c.release([t.b for t in xs])
            for kind_, layer in plan:
                {"hgrn": hgrn_phase, "dsa": dsa_phase, "ffn": ffn_phase}[kind_](layer)
            with ExitStack() as ps_:
                yf = sb(ps_, "yf", [128, 8, ST]); otok = [sb(ps_, f"otok{i}", [128, D]) for i in range(2)]
                it = 0
                for st in range(NST):
                    ts = slice(st * ST, (st + 1) * ST)
                    for dc in range(8):
                        s_ = sq[dc % 2]
                        op("act", lambda e: e.activation(s_.t[:], hT[:, dc, ts], AF.Square), reads=[bh[st]], writes=[s_.b])
                        op("pe", lambda e: e.matmul(P[:, 3, :], onesb.t[:], s_.t[:], start=(dc == 0), stop=(dc == 7)),
                           reads=[onesb.b, s_.b], writes=[pb[3]], pe_accum=True)
                    op("act", lambda e: e.activation(rstd.t[:], P[:, 3, :], AF.Sqrt, bias=EPS, scale=1.0 / D), reads=[pb[3]], writes=[rstd.b])
                    op("dve", lambda e: e.reciprocal(rstd.t[:], rstd.t[:]), reads=[rstd.b], writes=[rstd.b])
                    for dc in range(8):
                        op("dve", lambda e: e.scalar_tensor_tensor(yf.t[:, dc, :], hT[:, dc, ts], vec.t[:, 64 + dc:65 + dc], rstd.t[:], ALU.mult, ALU.mult),
                           reads=[bh[st], vec.b, rstd.b], writes=[yf.b])
                    for n in range(4):
                        ot = otok[it % 2]; it += 1
                        for dc in range(8):
                            op("pe", lambda e: e.transpose(P[:, dc // 4, (dc % 4) * 128:(dc % 4 + 1) * 128], yf.t[:, dc, n * 128:(n + 1) * 128], ident.t[:]),
                               reads=[yf.b, ident.b], writes=[pb[dc // 4]], pe_accum=True)
                        op("act", lambda e: e.copy(ot.t[:, 0:512], P[:, 0, :]), reads=[pb[0]], writes=[ot.b])
                        op("dve", lambda e: e.tensor_copy(ot.t[:, 512:1024], P[:, 1, :]), reads=[pb[1]], writes=[ot.b])
                        r0 = st * ST + n * 128
                        c.dma("sp", out[s, r0:r0 + 128, :], ot.t[:], reads=[ot.b])
                c.release([yf.b] + [t.b for t in otok])
        for k in range(len(c.dsem)):
            if c.dval[k] > 0:
                c._wait("sp", ("d", k, c.dval[k]))
        for eng in ("pe", "act", "dve", "pool"):
            if c.cnt[eng] > 0:
                c._wait("sp", ("e", eng, c.cnt[eng]))
        print("instr counts", c.cnt, "waits", c.nwaits, c.wstat)
    return nc


_NC_CACHE = {}


def kernel(**inputs):
    n = 8
    if "nc" not in _NC_CACHE:
        _NC_CACHE["nc"] = build_nc()
    nc = _NC_CACHE["nc"]
    consts = _host_consts()
    x = np.ascontiguousarray(np.asarray(inputs["x"], dtype=np.float32))
    shared = {k: np.ascontiguousarray(np.asarray(v, dtype=np.float32)) for k, v in inputs.items() if k != "x"}
    in_maps = []
    for i in range(n):
        m = dict(shared)
        m.update(consts)
        m["x"] = x[NSEQ * i:NSEQ * (i + 1)]
        in_maps.append(m)
    res = run_bass_kernel_spmd(nc, in_maps, core_ids=list(range(n)))
    return np.concatenate([np.asarray(r["out"]) for r in res.results], axis=0).astype(np.float32)
```
